# Optimizing a Trainium2 kernel written in Bass

```python
import math
import jax, jax.numpy as jnp
from jax import lax
import numpy as np

D_MODEL = 1024
BATCH = 32
SEQ = 2048
DEPTH = 1

HEAD_DIM = 64
NSA_HEADS = 8
NSA_KV_GROUPS = 2
NSA_REP = NSA_HEADS // NSA_KV_GROUPS
CMP_BLOCK = 32
CMP_STRIDE = 16
CMP_HIDDEN = 256
SEL_BLOCK = 64
SEL_TOP = 16
WINDOW = 512
Q_BLOCK = 128
NSA_WIDTH = NSA_HEADS * HEAD_DIM
DN_HEADS = 8
DN_CONV = 4
DN_CHUNK = 64
DN_WIDTH = DN_HEADS * HEAD_DIM
N_EXPERTS = 32
TOP_K = 4
D_EXPERT = D_MODEL
SWIGLU_LIMIT = 7.0
SWIGLU_ALPHA = 1.702
N_BRANCH = 2
N_MOD = 6
EPS = 1e-6
MASK_VALUE = -1e30
FORCE_VALUE = 1e9
IN_SPLITS = (NSA_WIDTH, 6 * NSA_KV_GROUPS * HEAD_DIM, 3 * NSA_HEADS,
             3 * DN_WIDTH, DN_HEADS, DN_HEADS, DN_WIDTH, N_BRANCH * D_MODEL)
IN_WIDTH = sum(IN_SPLITS)

kernel_name = 'hybrid_nsa_deltanet_moe_block'


def rms_norm(x, g):
    xf = x.astype(jnp.float32)
    y = xf * lax.rsqrt(jnp.mean(xf * xf, axis=-1, keepdims=True) + EPS)
    return (y * g.astype(jnp.float32)).astype(x.dtype)


def l2_normalize(x):
    xf = x.astype(jnp.float32)
    return xf * lax.rsqrt(jnp.sum(xf * xf, axis=-1, keepdims=True) + EPS)


def masked_softmax(s, mask):
    s = jnp.where(mask, s.astype(jnp.float32), MASK_VALUE)
    p = jax.nn.softmax(s, axis=-1)
    return jnp.where(mask, p, 0.0)


def split_columns(x, sizes):
    out, off = [], 0
    for s in sizes:
        out.append(x[..., off:off + s])
        off += s
    return out


def nsa_mixer(q, kv, gate_logits, pe_k, pe_v, cmp_w1, cmp_b1, cmp_w2):
    B, S = q.shape[:2]
    G, R, Dh = NSA_KV_GROUPS, NSA_REP, HEAD_DIM
    scale = Dh ** -0.5
    qg = q.reshape(B, S, G, R, Dh)
    k_cmp, v_cmp, k_sel, v_sel, k_win, v_win = [kv[:, :, i] for i in range(6)]
    t = jnp.arange(S)

    n_cmp = (S - CMP_BLOCK) // CMP_STRIDE + 1
    cmp_start = jnp.arange(n_cmp) * CMP_STRIDE
    gather_idx = cmp_start[:, None] + jnp.arange(CMP_BLOCK)[None, :]

    def compress(src, pe, w1, b1, w2):
        blk = src[:, gather_idx] + pe[:, None, :]
        blk = blk.transpose(0, 1, 3, 2, 4).reshape(B, n_cmp, G, CMP_BLOCK * Dh)
        return jax.nn.silu(blk @ w1 + b1) @ w2

    kc = compress(k_cmp, pe_k, cmp_w1[0], cmp_b1[0], cmp_w2[0])
    vc = compress(v_cmp, pe_v, cmp_w1[1], cmp_b1[1], cmp_w2[1])
    cmp_mask = (cmp_start + CMP_BLOCK - 1)[None, :] <= t[:, None]
    s_cmp = jnp.einsum('bsgrd,bngd->bgrsn', qg, kc) * scale
    p_cmp = masked_softmax(s_cmp, cmp_mask)
    o_cmp = jnp.einsum('bgrsn,bngd->bsgrd', p_cmp.astype(vc.dtype), vc).reshape(B, S, NSA_HEADS, Dh)

    n_sel = S // SEL_BLOCK
    sel_start = jnp.arange(n_sel) * SEL_BLOCK
    overlap = jnp.clip(jnp.minimum(cmp_start[:, None] + CMP_BLOCK, sel_start[None, :] + SEL_BLOCK)
                       - jnp.maximum(cmp_start[:, None], sel_start[None, :]), 0, None)
    overlap = overlap.astype(jnp.float32) / CMP_BLOCK
    imp = jnp.einsum('bgrsn,nj->bgsj', p_cmp, overlap)
    cur = t // SEL_BLOCK
    j = jnp.arange(n_sel)
    forced = (j[None, :] == 0) | (j[None, :] == cur[:, None]) | (j[None, :] == cur[:, None] - 1)
    causal = sel_start[None, :] <= t[:, None]
    score = jnp.where(forced, FORCE_VALUE, jnp.where(causal, imp, MASK_VALUE))
    n_top = min(SEL_TOP, n_sel)
    _, sel_idx = lax.top_k(score, n_top)

    k_sel_b = k_sel.reshape(B, n_sel, SEL_BLOCK, G, Dh).transpose(0, 3, 1, 2, 4)
    v_sel_b = v_sel.reshape(B, n_sel, SEL_BLOCK, G, Dh).transpose(0, 3, 1, 2, 4)
    pad = ((0, 0), (WINDOW, 0), (0, 0), (0, 0))
    k_win_p = jnp.pad(k_win, pad)
    v_win_p = jnp.pad(v_win, pad)
    b_i = jnp.arange(B)[:, None, None, None]
    g_i = jnp.arange(G)[None, :, None, None]
    n_keys = n_top * SEL_BLOCK

    def query_block(qb):
        start = qb * Q_BLOCK
        tq = start + jnp.arange(Q_BLOCK)
        q_blk = lax.dynamic_slice_in_dim(qg, start, Q_BLOCK, axis=1)
        idx = lax.dynamic_slice_in_dim(sel_idx, start, Q_BLOCK, axis=2)
        kg = k_sel_b[b_i, g_i, idx]
        vg = v_sel_b[b_i, g_i, idx]
        pos = idx[..., None] * SEL_BLOCK + jnp.arange(SEL_BLOCK)
        m_sel = (pos <= tq[:, None, None]).reshape(B, G, 1, Q_BLOCK, n_keys)
        s_sel = jnp.einsum('bqgrd,bgqnld->bgrqnl', q_blk, kg) * scale
        p_sel = masked_softmax(s_sel.reshape(B, G, R, Q_BLOCK, n_keys), m_sel)
        p_sel = p_sel.reshape(B, G, R, Q_BLOCK, n_top, SEL_BLOCK)
        o_sel = jnp.einsum('bgrqnl,bgqnld->bqgrd', p_sel.astype(vg.dtype), vg)
        kw = lax.dynamic_slice_in_dim(k_win_p, start, WINDOW + Q_BLOCK, axis=1)
        vw = lax.dynamic_slice_in_dim(v_win_p, start, WINDOW + Q_BLOCK, axis=1)
        kpos = start - WINDOW + jnp.arange(WINDOW + Q_BLOCK)
        delta = tq[:, None] - kpos[None, :]
        m_win = (delta >= 0) & (delta < WINDOW) & (kpos[None, :] >= 0)
        s_win = jnp.einsum('bqgrd,bkgd->bgrqk', q_blk, kw) * scale
        p_win = masked_softmax(s_win, m_win)
        o_win = jnp.einsum('bgrqk,bkgd->bqgrd', p_win.astype(vw.dtype), vw)
        return o_sel, o_win

    o_sel, o_win = lax.map(query_block, jnp.arange(S // Q_BLOCK))
    o_sel = o_sel.transpose(1, 0, 2, 3, 4, 5).reshape(B, S, NSA_HEADS, Dh)
    o_win = o_win.transpose(1, 0, 2, 3, 4, 5).reshape(B, S, NSA_HEADS, Dh)

    gates = jax.nn.sigmoid(gate_logits)
    o = gates[..., 0:1] * o_cmp + gates[..., 1:2] * o_sel + gates[..., 2:3] * o_win
    return o.reshape(B, S, NSA_WIDTH)


def causal_conv_silu(x, w):
    C = x.shape[-1]
    y = lax.conv_general_dilated(x, w[:, None, :].astype(x.dtype), window_strides=(1,),
                                 padding=[(DN_CONV - 1, 0)],
                                 dimension_numbers=('NWC', 'WIO', 'NWC'),
                                 feature_group_count=C)
    return jax.nn.silu(y)


def gated_delta_net(qkv, beta_logit, a_logit, z, a_log, dt_bias, norm_g):
    B, S, _ = qkv.shape
    H, Dh, C = DN_HEADS, HEAD_DIM, DN_CHUNK
    f32 = jnp.float32
    q, k, v = jnp.split(qkv.astype(f32), 3, axis=-1)
    q = l2_normalize(q.reshape(B, S, H, Dh)) * (Dh ** -0.5)
    k = l2_normalize(k.reshape(B, S, H, Dh))
    v = v.reshape(B, S, H, Dh)
    beta = jax.nn.sigmoid(beta_logit.astype(f32))
    g = -jnp.exp(a_log.astype(f32)) * jax.nn.softplus(a_logit.astype(f32) + dt_bias.astype(f32))
    nc = S // C

    def chunks(a):
        return a.reshape(B, nc, C, H, -1).transpose(1, 0, 3, 2, 4)

    q, k, v = chunks(q), chunks(k), chunks(v)
    beta = chunks(beta[..., None])[..., 0]
    gc = jnp.cumsum(chunks(g[..., None])[..., 0], axis=-1)
    diff = gc[..., :, None] - gc[..., None, :]
    causal = jnp.tril(jnp.ones((C, C), bool))
    strict = jnp.tril(jnp.ones((C, C), bool), -1)
    decay_in = jnp.exp(jnp.where(causal, diff, -jnp.inf))
    kb = k * beta[..., None]
    A = jnp.einsum('nbhid,nbhjd->nbhij', kb, k) * jnp.where(strict, decay_in, 0.0)
    rhs = jnp.concatenate([v * beta[..., None], kb * jnp.exp(gc)[..., None]], axis=-1)
    sol = lax.linalg.triangular_solve(A, rhs, left_side=True, lower=True, unit_diagonal=True)
    u, w = sol[..., :Dh], sol[..., Dh:]
    attn = jnp.einsum('nbhid,nbhjd->nbhij', q, k) * decay_in
    q_dec = q * jnp.exp(gc)[..., None]
    g_last = gc[..., -1]
    k_dec = k * jnp.exp(g_last[..., None] - gc)[..., None]

    def step(state, xs):
        w_c, u_c, q_c, k_c, a_c, gl = xs
        v_new = u_c - jnp.einsum('bhcd,bhde->bhce', w_c, state)
        o_c = jnp.einsum('bhcd,bhde->bhce', q_c, state) + jnp.einsum('bhij,bhje->bhie', a_c, v_new)
        state = state * jnp.exp(gl)[..., None, None] + jnp.einsum('bhcd,bhce->bhde', k_c, v_new)
        return state, o_c

    s0 = jnp.zeros((B, H, Dh, Dh), f32)
    _, o = lax.scan(step, s0, (w, u, q_dec, k_dec, attn, g_last))
    o = o.transpose(1, 0, 3, 2, 4).reshape(B, S, H, Dh)
    o = rms_norm(o, norm_g) * jax.nn.silu(z.reshape(B, S, H, Dh).astype(f32))
    return o.reshape(B, S, DN_WIDTH).astype(qkv.dtype)


def moe_ffn(h, router_w, router_b, w1, b1, w2, b2):
    B, S, D = h.shape
    hf = h.reshape(B * S, D)
    logits = (hf @ router_w + router_b).astype(jnp.float32)
    top_val, top_idx = lax.top_k(logits, TOP_K)
    top_w = jax.nn.softmax(top_val, axis=-1)
    combine = jnp.einsum('tk,tke->te', top_w,
                         jax.nn.one_hot(top_idx, N_EXPERTS, dtype=jnp.float32)).astype(h.dtype)
    out = jnp.zeros_like(hf)
    for e in range(N_EXPERTS):
        u = hf @ w1[e] + b1[e]
        x_glu = jnp.minimum(u[:, :D_EXPERT], SWIGLU_LIMIT)
        x_lin = jnp.clip(u[:, D_EXPERT:], -SWIGLU_LIMIT, SWIGLU_LIMIT)
        act = x_glu * jax.nn.sigmoid(SWIGLU_ALPHA * x_glu) * (x_lin + 1.0)
        out = out + combine[:, e:e + 1] * (act @ w2[e] + b2[e])
    return out.reshape(B, S, D)


def setup_inputs(seed: int = 0) -> dict:
    key = jax.random.key(seed)
    ks = jax.random.split(key, 32)
    L, D, E, F = DEPTH, D_MODEL, N_EXPERTS, D_EXPERT
    nrm = jax.random.normal
    f32 = jnp.float32
    dt = jnp.exp(jax.random.uniform(ks[12], (L, DN_HEADS), f32, math.log(1e-3), math.log(1e-1)))
    return {
        'x': nrm(ks[0], (BATCH, SEQ, D), f32),
        'c': nrm(ks[1], (BATCH, D), f32),
        'w_ada': nrm(ks[2], (L, D, N_MOD * D), f32) * (0.5 * D ** -0.5),
        'b_ada': nrm(ks[3], (L, N_MOD * D), f32) * 0.01,
        'g_norm_mix': 1.0 + 0.05 * nrm(ks[4], (L, D), f32),
        'w_in': nrm(ks[5], (L, D, IN_WIDTH), f32) * D ** -0.5,
        'cmp_pe_k': nrm(ks[6], (L, CMP_BLOCK, HEAD_DIM), f32) * 0.1,
        'cmp_pe_v': nrm(ks[7], (L, CMP_BLOCK, HEAD_DIM), f32) * 0.1,
        'cmp_w1': nrm(ks[8], (L, 2, CMP_BLOCK * HEAD_DIM, CMP_HIDDEN), f32) * (CMP_BLOCK * HEAD_DIM) ** -0.5,
        'cmp_b1': nrm(ks[9], (L, 2, CMP_HIDDEN), f32) * 0.01,
        'cmp_w2': nrm(ks[10], (L, 2, CMP_HIDDEN, HEAD_DIM), f32) * CMP_HIDDEN ** -0.5,
        'dn_conv_w': nrm(ks[11], (L, DN_CONV, 3 * DN_WIDTH), f32) * DN_CONV ** -0.5,
        'dn_a_log': jnp.log(jax.random.uniform(ks[13], (L, DN_HEADS), f32, 1.0, 16.0)),
        'dn_dt_bias': dt + jnp.log(-jnp.expm1(-dt)),
        'dn_norm_g': 1.0 + 0.05 * nrm(ks[14], (L, HEAD_DIM), f32),
        'w_branch': nrm(ks[15], (L, N_BRANCH, NSA_WIDTH, D), f32) * NSA_WIDTH ** -0.5,
        'w_out': nrm(ks[16], (L, D, D), f32) * D ** -0.5,
        'g_norm_ffn': 1.0 + 0.05 * nrm(ks[17], (L, D), f32),
        'router_w': nrm(ks[18], (L, D, E), f32) * D ** -0.5,
        'router_b': nrm(ks[19], (L, E), f32) * 0.01,
        'exp_w1': nrm(ks[20], (L, E, D, 2 * F), f32) * D ** -0.5,
        'exp_b1': nrm(ks[21], (L, E, 2 * F), f32) * 0.01,
        'exp_w2': nrm(ks[22], (L, E, F, D), f32) * F ** -0.5,
        'exp_b2': nrm(ks[23], (L, E, D), f32) * 0.01,
        'final_norm_g': 1.0 + 0.05 * nrm(ks[24], (D,), f32),
    }


def reference(x, c, w_ada, b_ada, g_norm_mix, w_in, cmp_pe_k, cmp_pe_v, cmp_w1, cmp_b1, cmp_w2,
              dn_conv_w, dn_a_log, dn_dt_bias, dn_norm_g, w_branch, w_out, g_norm_ffn,
              router_w, router_b, exp_w1, exp_b1, exp_w2, exp_b2, final_norm_g):
    B, S, D = x.shape
    for l in range(DEPTH):
        mod = jax.nn.silu(c) @ w_ada[l] + b_ada[l]
        shift_a, scale_a, gate_a, shift_f, scale_f, gate_f = [m[:, None, :] for m in jnp.split(mod, N_MOD, axis=-1)]

        h = rms_norm(x, g_norm_mix[l]) * (1.0 + scale_a) + shift_a
        proj = h @ w_in[l]
        nsa_q, nsa_kv, nsa_gate, dn_qkv, dn_beta, dn_a, dn_z, merge_logit = split_columns(proj, IN_SPLITS)
        y_nsa = nsa_mixer(nsa_q.reshape(B, S, NSA_HEADS, HEAD_DIM),
                          nsa_kv.reshape(B, S, 6, NSA_KV_GROUPS, HEAD_DIM),
                          nsa_gate.reshape(B, S, NSA_HEADS, 3),
                          cmp_pe_k[l], cmp_pe_v[l], cmp_w1[l], cmp_b1[l], cmp_w2[l])
        y_dn = gated_delta_net(causal_conv_silu(dn_qkv, dn_conv_w[l]), dn_beta, dn_a, dn_z,
                               dn_a_log[l], dn_dt_bias[l], dn_norm_g[l])
        branches = jnp.einsum('bsgc,gcd->bsgd', jnp.stack([y_nsa, y_dn], axis=2), w_branch[l])
        merge = jax.nn.sigmoid(merge_logit.reshape(B, S, N_BRANCH, D))
        mix = jnp.sum(merge * branches, axis=2) @ w_out[l]
        x = x + gate_a * mix

        h = rms_norm(x, g_norm_ffn[l]) * (1.0 + scale_f) + shift_f
        x = x + gate_f * moe_ffn(h, router_w[l], router_b[l], exp_w1[l], exp_b1[l], exp_w2[l], exp_b2[l])
    return rms_norm(x, final_norm_g)
```

```python
from contextlib import ExitStack
import numpy as np
import ml_dtypes
import concourse.bass as bass
import concourse.mybir as mybir
from concourse.bass_utils import run_bass_kernel_spmd

F32 = mybir.dt.float32
BF16 = mybir.dt.bfloat16
AF = mybir.ActivationFunctionType
ALU = mybir.AluOpType
AX = mybir.AxisListType

D = 1024
S = 2048
NT = S // 128
INW = 5416
NCORES = 8
BPC = 4
EPS = 1e-6
NEG = -30000.0
O_Q, O_KV, O_GATE, O_DNQKV, O_BETA, O_A, O_Z, O_MERGE = 0, 512, 1280, 1304, 2840, 2848, 2856, 3368


class Buf:
    def __init__(self, t, name):
        self.t = t
        self.name = name
        self.w = None
        self.r = {}
        self.dsem = None
        self.psum = False

    def __getitem__(self, k):
        return self.t[k]


class Prog:
    ENGS = ("pe", "dve", "act", "pool", "sp")

    def __init__(self):
        self.nc = bass.Bass("TRN2", target_bir_lowering=False)
        self.es = ExitStack()
        self.semstack = ExitStack()
        self.sems = {}
        self.semval = {}
        self.ops = {e: [] for e in self.ENGS}
        self.seen = {e: {} for e in self.ENGS}
        for e in self.ENGS:
            self._newsem("E_" + e)
        self.nbuf = 0
        self.stack = []
        self.uid = 0

    def _newsem(self, key):
        if key in self.sems:
            return key
        self.sems[key] = self.semstack.enter_context(self.nc.semaphore(key))
        self.semval[key] = 0
        return key

    def sb(self, name, shape, dt):
        self.uid += 1
        t = self.es.enter_context(self.nc.sbuf_tensor("s%d_%s" % (self.uid, name), list(shape), dt))
        return Buf(t, name)

    def ps(self, name, shape, dt):
        self.uid += 1
        t = self.es.enter_context(self.nc.psum_tensor("p%d_%s" % (self.uid, name), list(shape), dt))
        b = Buf(t, name)
        b.psum = True
        return b

    def dram(self, name, shape, dt, kind="Internal"):
        t = self.nc.dram_tensor(name, list(shape), dt, kind=kind).ap()
        return Buf(t, name)

    def _deps(self, eng, reads, writes):
        own = "E_" + eng
        deps = {}

        def add(tok):
            if tok is None:
                return
            k, v = tok
            if deps.get(k, 0) < v:
                deps[k] = v

        for b in reads:
            if b.w is not None:
                if b.w[0] == own and eng == "pe":
                    continue
                add(b.w)
            if b.psum:
                for k, v in b.r.items():
                    if k != own:
                        add((k, v))
        for b in writes:
            if b.w is not None and (b.w[0] != own or eng != "pe"):
                add(b.w)
            for k, v in b.r.items():
                if k != own or eng != "pe":
                    add((k, v))
        waits = []
        seen = self.seen[eng]
        for k, v in deps.items():
            if seen.get(k, 0) < v:
                waits.append((k, v))
                seen[k] = v
        return waits

    def _commit(self, tok, reads, writes):
        k, v = tok
        for b in reads:
            if b.r.get(k, 0) < v:
                b.r[k] = v
        for b in writes:
            b.w = tok
            b.r = {}

    def op(self, eng, fn, reads=(), writes=()):
        waits = self._deps(eng, reads, writes)
        own = "E_" + eng
        self.semval[own] += 1
        tok = (own, self.semval[own])
        self.seen[eng][own] = max(self.seen[eng].get(own, 0), 0)
        self._emit1(eng, waits, fn, [(own, 1)], False)
        self._commit(tok, reads, writes)

    def dma(self, eng, fn, reads=(), writes=(), n=1, sem=None):
        waits = self._deps(eng, reads, writes)
        if sem is None:
            tgt = writes[0]
            if tgt.dsem is None:
                tgt.dsem = self._newsem("D_" + tgt.name)
            sem = tgt.dsem
        self.semval[sem] += 16 * n
        tok = (sem, self.semval[sem])
        self._emit1(eng, waits, fn, [(sem, 16)] * n, True)
        self._commit(tok, reads, writes)
        return tok

    def wait_all(self, eng, toks):
        waits = [(k, v) for (k, v) in toks]
        self._emit1(eng, waits, None, [], False)

    def _emit1(self, eng, waits, fn, incs, isdma):
        nc = self.nc
        e = {"pe": nc.tensor, "dve": nc.vector, "act": nc.scalar, "pool": nc.gpsimd, "sp": nc.sync}[eng]
        sems = self.sems
        for k, v in waits:
            e.wait_ge(sems[k], v)
        if fn is None:
            return
        r = fn(e)
        if isdma:
            assert len(r) == len(incs), (len(r), len(incs))
            for ins, (k, a) in zip(r, incs):
                ins.then_inc(sems[k], a)
        else:
            r.then_inc(sems[incs[0][0]], 1)

    def barrier(self):
        for eng in self.ENGS:
            waits = []
            for k, v in self.semval.items():
                if v > 0 and k != "E_" + eng and self.seen[eng].get(k, 0) < v:
                    waits.append((k, v))
                    self.seen[eng][k] = v
            self._emit1(eng, waits, None, [], False)

    def push(self):
        self.stack.append(self.es)
        self.es = ExitStack()

    def pop(self):
        self.barrier()
        self.es.close()
        self.es = self.stack.pop()

    def emit(self):
        self.es.close()
        self.semstack.close()
        return

    def emit_old(self):
        nc = self.nc
        sems = self.sems
        with nc.Block() as block:
            def mk(eng):
                lst = self.ops[eng]

                def body(e):
                    for waits, fn, incs, isdma in lst:
                        for k, v in waits:
                            e.wait_ge(sems[k], v)
                        if fn is None:
                            continue
                        r = fn(e)
                        if isdma:
                            assert len(r) == len(incs), (len(r), len(incs))
                            for ins, (k, a) in zip(r, incs):
                                ins.then_inc(sems[k], a)
                        else:
                            r.then_inc(sems[incs[0][0]], 1)
                return body
            block.tensor(mk("pe"))
            block.vector(mk("dve"))
            block.scalar(mk("act"))
            block.gpsimd(mk("pool"))
            block.sync(mk("sp"))
        self.es.close()


def bf(x):
    return np.asarray(x, dtype=np.float32).astype(ml_dtypes.bfloat16)


SIGMAX = float(1.0 / (1.0 + np.exp(-1.702 * 7.0)))


def host_consts():
    c = {}
    c["ident_bf"] = bf(np.eye(128))
    c["ident_f"] = np.eye(128, dtype=np.float32)
    c["ones_f"] = np.ones((128, 128), np.float32)
    i = np.arange(64)
    tri = (i[:, None] <= i[None, :]).astype(np.float32)
    c["tri64"] = tri
    c["maskpos"] = np.where(i[:, None] >= i[None, :], 0.0, 1.0e4).astype(np.float32)
    strict = (i[:, None] > i[None, :]).astype(np.float32)
    c["strict8"] = np.ascontiguousarray(np.broadcast_to(strict[:, None, :], (64, 8, 64))).reshape(64, 512)
    t = np.arange(S)
    n = np.arange(127)
    c["negcmp"] = bf(np.where((16 * n[:, None] + 31) <= t[None, :], 0.0, NEG))
    kk = np.arange(128)
    diag = np.where(kk[:, None] <= kk[None, :], 0.0, NEG)
    far = np.where(kk[:, None] > kk[None, :], 0.0, NEG)
    c["negdiag4"] = bf(np.tile(diag, (1, 4)))
    c["negfar4"] = bf(np.tile(far, (1, 4)))
    j = np.arange(32)
    c["e32"] = bf((t[None, :] // 64 == j[:, None]).astype(np.float32))
    cur = t // 64
    forced = (j[None, :] == 0) | (j[None, :] == cur[:, None]) | (j[None, :] == cur[:, None] - 1)
    causal = (64 * j[None, :]) <= t[:, None]
    m1 = np.where(forced, 0.0, np.where(causal, 1.0, 0.0)).astype(np.float32)
    m2 = np.where(forced, 1.0e9, np.where(causal, 0.0, -1.0e30)).astype(np.float32)
    c["selm1"] = np.ascontiguousarray(m1.reshape(16, 128, 32).transpose(1, 0, 2))
    c["selm2"] = np.ascontiguousarray(m2.reshape(16, 128, 32).transpose(1, 0, 2))
    ov = np.clip(np.minimum(16 * n[:, None] + 32, 64 * j[None, :] + 64) - np.maximum(16 * n[:, None], 64 * j[None, :]), 0, None) / 32.0
    cmpaug = np.zeros((127, 33), np.float32)
    cmpaug[:, 0] = 1.0
    cmpaug[:, 1:33] = ov
    c["cmpaug"] = cmpaug
    c["eye8"] = np.ascontiguousarray(np.broadcast_to(np.eye(64, dtype=np.float32)[:, None, :], (64, 8, 64))).reshape(64, 512)
    return c


def build(nseq=BPC, dbg=None, inject=False, moe_nexp=32, moe_ntc=None, moe_cut=9, inject_nsa=False, inject_dn=False, dn_nchunk=32, nsa_nqt=16):
    P = Prog()
    nc = P.nc
    IN = lambda name, shape, dt=F32: nc.dram_tensor(name, list(shape), dt, kind="ExternalInput").ap()
    x_d = IN("x", [nseq, S, D])
    c_d = IN("c", [nseq, D])
    w_ada_d = IN("w_ada", [D, 6 * D])
    b_ada_d = IN("b_ada", [1, 6 * D])
    g_mix_d = IN("g_norm_mix", [1, D])
    g_ffn_d = IN("g_norm_ffn", [1, D])
    g_fin_d = IN("final_norm_g", [1, D])
    w_in_d = IN("w_in", [D, INW])
    w_br_d = IN("w_branch", [2, 512, D])
    w_out_d = IN("w_out", [D, D])
    rw_d = IN("router_w", [D, 32])
    rb_d = IN("router_b", [1, 32])
    ew1_d = IN("exp_w1", [32, D, 2 * D])
    eb1_d = IN("exp_b1", [32, 2 * D])
    ew2_d = IN("exp_w2", [32, D, D])
    eb2_d = IN("exp_b2", [32, D])
    convw_d = IN("dn_conv_w", [1, 4 * 1536])
    alog_d = IN("dn_a_log", [1, 8])
    dtb_d = IN("dn_dt_bias", [1, 8])
    dng_d = IN("dn_norm_g", [1, 64])
    pek_d = IN("cmp_pe_k", [32, 64])
    pev_d = IN("cmp_pe_v", [32, 64])
    cw1_d = IN("cmp_w1", [2, 2048, 256])
    cb1_d = IN("cmp_b1", [2, 256])
    cw2_d = IN("cmp_w2", [2, 256, 64])
    negcmp_d = IN("negcmp", [127, S], BF16)
    negdiag4_d = IN("negdiag4", [128, 512], BF16)
    negfar4_d = IN("negfar4", [128, 512], BF16)
    e32_d = IN("e32", [32, S], BF16)
    selm1_d = IN("selm1", [128, 16, 32])
    selm2_d = IN("selm2", [128, 16, 32])
    cmpaug_d = IN("cmpaug", [127, 33])
    tri64_d = IN("tri64", [64, 64])
    maskpos_d = IN("maskpos", [64, 64])
    strict8_d = IN("strict8", [64, 512])
    eye8_d = IN("eye8", [64, 512])
    ident_bf_d = IN("ident_bf", [128, 128], BF16)
    ident_f_d = IN("ident_f", [128, 128])
    ones_f_d = IN("ones_f", [128, 128])
    inject_nsa = inject_nsa or inject
    inject_dn = inject_dn or inject
    if inject_nsa:
        ynsa_in = IN("ynsa_in", [S, 512])
    if inject_dn:
        ydn_in = IN("ydn_in", [S, 512])
    out_d = nc.dram_tensor("out", [nseq, S, D], F32, kind="ExternalOutput").ap()
    dbg_d = None
    if dbg == "proj":
        dbg_d = nc.dram_tensor("dbg", [S, INW], F32, kind="ExternalOutput").ap()
    elif dbg == "x1":
        dbg_d = nc.dram_tensor("dbg", [S, D], F32, kind="ExternalOutput").ap()
    elif dbg in ("y_dn", "y_nsa"):
        dbg_d = nc.dram_tensor("dbg", [S, 512], F32, kind="ExternalOutput").ap()

    ident_bf = P.sb("ident_bf", [128, 128], BF16)
    ident_f = P.sb("ident_f", [128, 128], F32)
    ones_f = P.sb("ones_f", [128, 128], F32)
    cT = P.sb("cT", [128, nseq, 8], F32)
    sc = P.sb("sc", [128, nseq, 8], F32)
    modF = P.sb("modF", [128, 3 * D], F32)

    def ld_consts(e):
        r = []
        r.append(e.dma_start(out=ident_bf[:], in_=ident_bf_d[:, :]))
        r.append(e.dma_start(out=ident_f[:], in_=ident_f_d[:, :]))
        r.append(e.dma_start(out=ones_f[:], in_=ones_f_d[:, :]))
        for b in range(nseq):
            r.append(e.dma_start(out=cT[:, b, :], in_=c_d[b].rearrange("(k p) -> p k", p=128),
                                 allow_slow_non_contiguous=True))
        return r
    P.dma("sp", ld_consts, writes=[ident_bf, ident_f, ones_f, cT], n=3 + nseq)
    P.op("act", lambda e: e.activation(sc[:], cT[:], AF.Silu), reads=[cT], writes=[sc])

    psA = [P.ps("psA%d" % i, [128, 512], F32) for i in range(6)]
    proj_d = P.dram("proj_scr", [S, INW], F32)
    ynsa_d = P.dram("ynsa_scr", [S, 512], F32)
    ydn_d = P.dram("ydn_scr", [S, 512], F32)
    x1_d = P.dram("x1_scr", [S, D], F32)
    if inject_nsa:
        ynsa_d = Buf(ynsa_in, "ynsa_in")
    if inject_dn:
        ydn_d = Buf(ydn_in, "ydn_in")
    out_b = Buf(out_d, "outd")

    w_ada_v = w_ada_d.rearrange("(k p) n -> p k n", p=128)
    w_in_v = w_in_d.rearrange("(k p) n -> p k n", p=128)

    def rmsnorm_mod(src, grow_a, shift_ap, ssb, rsb, tmp, dst, shift_buf=None):
        P.op("act", lambda e: e.activation(tmp[:], src[:], AF.Square, accum_out=ssb[:]), reads=[src], writes=[tmp, ssb])
        P.op("dve", lambda e: e.tensor_scalar(rsb[:], ssb[:], 1.0 / D, EPS, ALU.mult, ALU.add), reads=[ssb], writes=[rsb])
        P.op("act", lambda e: e.activation(ssb[:], rsb[:], AF.Sqrt), reads=[rsb], writes=[ssb])
        P.op("dve", lambda e: e.reciprocal(rsb[:], ssb[:]), reads=[ssb], writes=[rsb])
        P.op("dve", lambda e: e.scalar_tensor_tensor(tmp[:], src[:], rsb[:, 0:1], grow_a[:], ALU.mult, ALU.mult),
             reads=[src, rsb, grow_a], writes=[tmp])
        if shift_ap is None:
            P.op("dve", lambda e: e.tensor_copy(dst[:], tmp[:]), reads=[tmp], writes=[dst])
        else:
            P.op("dve", lambda e: e.tensor_tensor(dst[:], tmp[:], shift_ap(), ALU.add), reads=[tmp, shift_buf], writes=[dst])

    def transpose8(src_bf, ptb, dst_ap_fn, dst_buf, eng="act"):
        def tr(e):
            for k in range(8):
                r = e.transpose(ptb[:, k, :], src_bf[:, k * 128:(k + 1) * 128], ident_bf[:])
            return r
        P.op("pe", tr, reads=[src_bf, ident_bf], writes=[ptb])
        if eng == "act":
            P.op("act", lambda e: e.copy(dst_ap_fn(), ptb[:]), reads=[ptb], writes=[dst_buf])
        else:
            P.op("dve", lambda e: e.tensor_copy(dst_ap_fn(), ptb[:]), reads=[ptb], writes=[dst_buf])

    for b in range(nseq):
        P.push()
        modA = P.sb("modA", [128, 3 * D], F32)
        P.push()
        psT = [P.ps("psT%d" % i, [128, 8, 128], BF16) for i in range(2)]
        g_mix_row = P.sb("g_mix_row", [128, D], F32)
        P.dma("sp", lambda e: [e.dma_start(out=g_mix_row[:], in_=g_mix_d.partition_broadcast(128))], writes=[g_mix_row])
        b_ada_sb = [P.sb("b_ada_sb%d" % i, [1, 256], F32) for i in range(2)]
        arow = P.sb("arow", [128, D], F32)
        scb = P.sb("scb", [128, 8, 128], F32)
        wada = [P.sb("wada%d" % i, [128, 8, 256], F32) for i in range(2)]
        hT = P.sb("hT", [128, 8, 4 + S], BF16)
        P.op("dve", lambda e: e.memset(hT[:, :, 0:4], 0.0), writes=[hT])
        cwrow = P.sb("cwrow", [128, 4 * 1536], F32)
        P.dma("sp", lambda e: [e.dma_start(out=cwrow[:], in_=convw_d.partition_broadcast(128))], writes=[cwrow])
        wtap = [P.sb("wtap%d" % j, [128, 8, 512], BF16) for j in range(4)]
        xt = [P.sb("xt%d" % i, [128, D], F32) for i in range(2)]
        ss = [P.sb("ss%d" % i, [128, 1], F32) for i in range(2)]
        rstd = [P.sb("rstd%d" % i, [128, 1], F32) for i in range(2)]
        xm = [P.sb("xm%d" % i, [128, D], F32) for i in range(2)]
        hb = [P.sb("hb%d" % i, [128, D], BF16) for i in range(2)]
        wblk = [P.sb("wblk%d" % i, [128, 8, 512], BF16) for i in range(2)]
        pst = [P.sb("pst%d" % i, [128, 512], F32) for i in range(3)]

        P.op("dve", lambda e, b=b: e.tensor_copy(scb[:], sc[:, b, :].unsqueeze(2).to_broadcast([128, 8, 128])),
             reads=[sc], writes=[scb])
        for j in range(24):
            wb_ = wada[j % 2]
            bb_ = b_ada_sb[j % 2]
            P.dma("sp", lambda e, j=j, wb_=wb_: [e.dma_start(out=wb_[:], in_=w_ada_v[:, :, j * 256:(j + 1) * 256])], writes=[wb_])
            P.dma("sp", lambda e, j=j, bb_=bb_: [e.dma_start(out=bb_[:], in_=b_ada_d[:, j * 256:(j + 1) * 256])], writes=[bb_])
            pb = psA[j % 2]

            def mm(e, j=j, wb_=wb_, pb=pb, bb_=bb_):
                for k in range(8):
                    e.matmul(pb[:, 0:256], scb[:, k, :], wb_[:, k, :], start=(k == 0), stop=False)
                return e.matmul(pb[:, 0:256], ones_f[0:1, :], bb_[0:1, :], start=False, stop=True)
            P.op("pe", mm, reads=[scb, wb_, ones_f, bb_], writes=[pb])
            mdst = modA if j < 12 else modF
            P.op("act", lambda e, j=j, pb=pb, mdst=mdst: e.copy(mdst[:, (j % 12) * 256:(j % 12 + 1) * 256], pb[:, 0:256]), reads=[pb], writes=[mdst])
        P.op("dve", lambda e: e.scalar_tensor_tensor(arow[:], modA[:, D:2 * D], 1.0, g_mix_row[:], ALU.add, ALU.mult),
             reads=[modA, g_mix_row], writes=[arow])

        for t in range(NT):
            i = t % 2
            P.dma("sp", lambda e, b=b, t=t, i=i: [e.dma_start(out=xt[i][:], in_=x_d[b, t * 128:(t + 1) * 128, :])], writes=[xt[i]])
            rmsnorm_mod(xt[i], arow, lambda: modA[:, 0:D], ss[i], rstd[i], xm[i], hb[i], modA)
            transpose8(hb[i], psT[i], lambda t=t: hT[:, :, 4 + t * 128:4 + (t + 1) * 128], hT)

        blocks = [(0, 512, False), (512, 512, False), (1024, O_DNQKV - 1024, False)]
        blocks += [(O_DNQKV + i * 512, 512, True) for i in range(3)]
        c0 = O_BETA
        while c0 < INW:
            blocks.append((c0, min(512, INW - c0), False))
            c0 += 512
        cnt = 0
        for j, (c0, cw, isdn) in enumerate(blocks):
            wb_ = wblk[j % 2]
            P.dma("pool", lambda e, wb_=wb_, c0=c0, cw=cw: [e.dma_start(out=wb_[:, :, 0:cw], in_=w_in_v[:, :, c0:c0 + cw])], writes=[wb_])
            if isdn:
                for tp in range(4):
                    cc = tp * 1536 + (c0 - O_DNQKV)
                    P.op("dve", lambda e, tp=tp, wb_=wb_, cc=cc: e.tensor_tensor(
                        wtap[tp][:], wb_[:], cwrow[:, cc:cc + 512].unsqueeze(1).to_broadcast([128, 8, 512]), ALU.mult),
                        reads=[wb_, cwrow], writes=[wtap[tp]])
            for t in range(NT):
                pb = psA[cnt % 4]
                sbt = pst[cnt % 3]
                cnt += 1
                if isdn:
                    def mm(e, t=t, pb=pb):
                        n = 0
                        for tp in range(4):
                            for k in range(8):
                                lo = 4 + t * 128 - 3 + tp
                                r = e.matmul(pb[:, 0:512], hT[:, k, lo:lo + 128], wtap[tp][:, k, :], start=(n == 0), stop=(n == 31))
                                n += 1
                        return r
                    P.op("pe", mm, reads=[hT] + wtap, writes=[pb])
                else:
                    def mm(e, t=t, wb_=wb_, pb=pb, cw=cw):
                        for k in range(8):
                            r = e.matmul(pb[:, 0:cw], hT[:, k, 4 + t * 128:4 + (t + 1) * 128], wb_[:, k, 0:cw], start=(k == 0), stop=(k == 7))
                        return r
                    P.op("pe", mm, reads=[hT, wb_], writes=[pb])
                if cnt % 2 == 0:
                    P.op("act", lambda e, pb=pb, sbt=sbt, cw=cw: e.copy(sbt[:, 0:cw], pb[:, 0:cw]), reads=[pb], writes=[sbt])
                else:
                    P.op("dve", lambda e, pb=pb, sbt=sbt, cw=cw: e.tensor_copy(sbt[:, 0:cw], pb[:, 0:cw]), reads=[pb], writes=[sbt])
                P.dma("sp", lambda e, t=t, sbt=sbt, c0=c0, cw=cw: [e.dma_start(out=proj_d[t * 128:(t + 1) * 128, c0:c0 + cw], in_=sbt[:, 0:cw])],
                      reads=[sbt], writes=[proj_d])
        P.pop()

        if not inject_dn:
            dn_stage(P, locals())
            if dbg == "y_dn":
                otok = P.dma("sp", lambda e: [e.dma_start(out=dbg_d[:, :], in_=ydn_d[:, :])], reads=[ydn_d], writes=[], sem=P._newsem("D_dbg"))
                P.wait_all("sp", [otok])
                break
        if not inject_nsa:
            nsa_stage(P, locals())
            if dbg == "y_nsa":
                otok = P.dma("sp", lambda e: [e.dma_start(out=dbg_d[:, :], in_=ynsa_d[:, :])], reads=[ynsa_d], writes=[], sem=P._newsem("D_dbg"))
                P.wait_all("sp", [otok])
                break

        P.push()
        psT = [P.ps("psT%d" % i, [128, 8, 128], BF16) for i in range(2)]
        wbr = P.sb("wbr", [128, 8, D], BF16)
        wout = P.sb("wout", [128, 8, D], BF16)
        P.dma("pool", lambda e: [e.dma_start(out=wbr[:, 0:4, :], in_=w_br_d[0].rearrange("(k p) n -> p k n", p=128)),
                                 e.dma_start(out=wbr[:, 4:8, :], in_=w_br_d[1].rearrange("(k p) n -> p k n", p=128))],
              writes=[wbr], n=2)
        P.dma("pool", lambda e: [e.dma_start(out=wout[:], in_=w_out_d.rearrange("(k p) n -> p k n", p=128))], writes=[wout])
        yin = [P.sb("yin%d" % i, [128, D], F32) for i in range(2)]
        ybf = [P.sb("ybf%d" % i, [128, D], BF16) for i in range(2)]
        yT = [P.sb("yT%d" % i, [128, 8, 128], BF16) for i in range(2)]
        mlog = [P.sb("mlog%d" % i, [128, 2 * D], F32) for i in range(2)]
        mix = [P.sb("mix%d" % i, [128, D], F32) for i in range(2)]
        mixb = [P.sb("mixb%d" % i, [128, D], BF16) for i in range(2)]
        mT = [P.sb("mT%d" % i, [128, 8, 128], BF16) for i in range(2)]
        xr = [P.sb("xr%d" % i, [128, D], F32) for i in range(2)]
        x1t = [P.sb("x1t%d" % i, [128, D], F32) for i in range(2)]
        tmpm = P.sb("tmpm", [128, 512], F32)
        for t in range(NT):
            i = t % 2
            rows = slice(t * 128, (t + 1) * 128)
            P.dma("sp", lambda e, i=i, rows=rows: [e.dma_start(out=yin[i][:, 0:512], in_=ynsa_d[rows, :]),
                                                    e.dma_start(out=yin[i][:, 512:1024], in_=ydn_d[rows, :])],
                  reads=[ynsa_d, ydn_d], writes=[yin[i]], n=2)
            P.dma("sp", lambda e, i=i, rows=rows: [e.dma_start(out=mlog[i][:], in_=proj_d[rows, O_MERGE:O_MERGE + 2 * D])],
                  reads=[proj_d], writes=[mlog[i]])
            P.dma("sp", lambda e, i=i, rows=rows, b=b: [e.dma_start(out=xr[i][:], in_=x_d[b, rows, :])], writes=[xr[i]])
            P.op("dve", lambda e, i=i: e.tensor_copy(ybf[i][:], yin[i][:]), reads=[yin[i]], writes=[ybf[i]])
            transpose8(ybf[i], psT[i], lambda i=i: yT[i][:], yT[i], eng="dve")
            P.op("act", lambda e, i=i: e.activation(mlog[i][:], mlog[i][:], AF.Sigmoid), reads=[mlog[i]], writes=[mlog[i]])
            for half in range(2):
                cs = slice(half * 512, (half + 1) * 512)
                for br in range(2):
                    pb = psA[half * 2 + br]

                    def mm(e, i=i, br=br, pb=pb, cs=cs):
                        for k in range(4):
                            r = e.matmul(pb[:], yT[i][:, br * 4 + k, :], wbr[:, br * 4 + k, cs], start=(k == 0), stop=(k == 3))
                        return r
                    P.op("pe", mm, reads=[yT[i], wbr], writes=[pb])
                P.op("dve", lambda e, i=i, cs=cs, half=half: e.tensor_tensor(mix[i][:, cs], psA[half * 2][:], mlog[i][:, cs], ALU.mult),
                     reads=[psA[half * 2], mlog[i]], writes=[mix[i]])
                P.op("dve", lambda e, i=i, half=half: e.tensor_tensor(tmpm[:], psA[half * 2 + 1][:], mlog[i][:, D + half * 512:D + (half + 1) * 512], ALU.mult),
                     reads=[psA[half * 2 + 1], mlog[i]], writes=[tmpm])
                P.op("dve", lambda e, i=i, cs=cs: e.tensor_tensor(mixb[i][:, cs], mix[i][:, cs], tmpm[:], ALU.add),
                     reads=[mix[i], tmpm], writes=[mixb[i]])
            transpose8(mixb[i], psT[i], lambda i=i: mT[i][:], mT[i], eng="act")
            for half in range(2):
                cs = slice(half * 512, (half + 1) * 512)
                pb = psA[4 + half]

                def mm(e, i=i, pb=pb, cs=cs):
                    for k in range(8):
                        r = e.matmul(pb[:], mT[i][:, k, :], wout[:, k, cs], start=(k == 0), stop=(k == 7))
                    return r
                P.op("pe", mm, reads=[mT[i], wout], writes=[pb])
                P.op("dve", lambda e, i=i, pb=pb, cs=cs, half=half: e.tensor_tensor(
                    x1t[i][:, cs], pb[:], modA[:, 2 * D + half * 512:2 * D + (half + 1) * 512], ALU.mult),
                    reads=[pb, modA], writes=[x1t[i]])
            P.op("dve", lambda e, i=i: e.tensor_tensor(x1t[i][:], x1t[i][:], xr[i][:], ALU.add), reads=[x1t[i], xr[i]], writes=[x1t[i]])
            P.dma("sp", lambda e, i=i, rows=rows: [e.dma_start(out=x1_d[rows, :], in_=x1t[i][:])], reads=[x1t[i]], writes=[x1_d])
        P.pop()
        P.pop()
        if dbg == "x1":
            otok = P.dma("sp", lambda e: [e.dma_start(out=dbg_d[:, :], in_=x1_d[:, :])], reads=[x1_d], writes=[], sem=P._newsem("D_dbg"))
            P.wait_all("sp", [otok])
            break

        P.push()
        psT = [P.ps("psT%d" % i, [128, 8, 128], BF16) for i in range(2)]
        frow = P.sb("frow", [128, D], F32)
        rb_row = P.sb("rb_row", [128, 32], F32)
        rw = P.sb("rw", [128, 8, 32], BF16)
        P.dma("sp", lambda e: [e.dma_start(out=rb_row[:], in_=rb_d.partition_broadcast(128))], writes=[rb_row])
        P.dma("pool", lambda e: [e.dma_start(out=rw[:], in_=rw_d.rearrange("(k p) n -> p k n", p=128))], writes=[rw])
        TC = 1024
        NTC = S // TC
        NTT = TC // 128
        NSUB = TC // 512
        acc = P.sb("acc", [128, NTT, D], F32)
        h2T = P.sb("h2T", [128, 8, TC], BF16)
        x1l = [P.sb("x1l0", [128, D], F32)] * 2
        ss2 = [P.sb("ssb%d" % i, [128, 1], F32) for i in range(2)]
        rs2 = [P.sb("rsb%d" % i, [128, 1], F32) for i in range(2)]
        tm2 = [P.sb("tm20", [128, D], F32)] * 2
        h2b = [P.sb("h2b0", [128, D], BF16)] * 2
        comb = P.sb("comb", [128, NTT, 32], F32)
        lg = P.sb("lg", [128, 32], F32)
        m8 = P.sb("m8", [128, 8], F32)
        nmx = P.sb("nmx", [128, 1], F32)
        msk = P.sb("msk", [128, 32], F32)
        esum = P.sb("esum", [128, 1], F32)
        combT = P.sb("combT", [32, 128], F32)
        w1s = [P.sb("w1s%d" % i, [128, 8, 2 * D], BF16) for i in range(2)]
        w2s = [P.sb("w2s0", [128, 8, D], BF16)] * 2
        b1all = P.sb("b1all", [128, 512], F32)
        b1raw = P.sb("b1raw", [128, 128], F32)
        eb1_v = eb1_d.rearrange("e (c p) -> (e c) p", p=128)
        for q in range(4):
            P.dma("sp", lambda e, q=q: [e.dma_start(out=b1raw[:], in_=eb1_v[q * 128:(q + 1) * 128, :])], writes=[b1raw])
            P.op("pe", lambda e: e.transpose(psA[0][:, 0:128], b1raw[:], ident_f[:]), reads=[b1raw, ident_f], writes=[psA[0]])
            P.op("act", lambda e, q=q: e.copy(b1all[:, q * 128:(q + 1) * 128], psA[0][:, 0:128]), reads=[psA[0]], writes=[b1all])
        b1s = [P.sb("b1s%d" % i, [128, 8], F32) for i in range(2)]
        b1l = [P.sb("b1l%d" % i, [128, 8], F32) for i in range(2)]
        b2s = P.sb("b2s", [32, D], F32)
        c7 = P.sb("c7", [128, 512], BF16)
        c8 = P.sb("c8", [128, 512], BF16)
        P.op("dve", lambda e: e.memset(c7[:], 7.0), writes=[c7])
        P.op("dve", lambda e: e.memset(c8[:], 8.0), writes=[c8])
        P.dma("sp", lambda e: [e.dma_start(out=b2s[:], in_=eb2_d[:, :])], writes=[b2s])
        actT = [P.sb("actT%d" % i, [128, 8, 512], BF16) for i in range(2)]
        sg = [P.sb("sg0", [128, 512], F32)] * 2
        gg = [P.sb("gg%d" % i, [128, 512], F32) for i in range(2)]
        ll = [P.sb("ll0", [128, 512], F32)] * 2
        fin = x1l
        ew1_v = ew1_d.rearrange("e (k p) n -> e p k n", p=128)
        ew2_v = ew2_d.rearrange("e (k p) n -> e p k n", p=128)
        for tc in range(NTC if moe_ntc is None else moe_ntc):
            P.dma("sp", lambda e: [e.dma_start(out=frow[:], in_=g_ffn_d.partition_broadcast(128))], writes=[frow])
            P.op("dve", lambda e: e.scalar_tensor_tensor(frow[:], modF[:, D:2 * D], 1.0, frow[:], ALU.add, ALU.mult),
                 reads=[modF, frow], writes=[frow])
            for tt in range(NTT):
                i = tt % 2
                rows = slice(tc * TC + tt * 128, tc * TC + (tt + 1) * 128)
                P.dma("sp", lambda e, i=i, rows=rows: [e.dma_start(out=x1l[i][:], in_=x1_d[rows, :])], reads=[x1_d], writes=[x1l[i]])
                rmsnorm_mod(x1l[i], frow, lambda: modF[:, 0:D], ss2[i], rs2[i], tm2[i], h2b[i], modF)
                transpose8(h2b[i], psT[i], lambda tt=tt: h2T[:, :, tt * 128:(tt + 1) * 128], h2T)
                pb = psA[4 + i]

                def mmr(e, tt=tt, pb=pb):
                    for k in range(8):
                        r = e.matmul(pb[:, 0:32], h2T[:, k, tt * 128:(tt + 1) * 128], rw[:, k, :], start=(k == 0), stop=(k == 7))
                    return r
                P.op("pe", mmr, reads=[h2T, rw], writes=[pb])
                P.op("dve", lambda e, pb=pb: e.tensor_tensor(lg[:], pb[:, 0:32], rb_row[:], ALU.add), reads=[pb, rb_row], writes=[lg])
                P.op("dve", lambda e: e.max(m8[:], lg[:]), reads=[lg], writes=[m8])
                P.op("dve", lambda e: e.tensor_scalar(msk[:], lg[:], m8[:, 3:4], None, ALU.is_ge), reads=[lg, m8], writes=[msk])
                P.op("dve", lambda e: e.tensor_scalar(nmx[:], m8[:, 0:1], -1.0, None, ALU.mult), reads=[m8], writes=[nmx])
                P.op("act", lambda e: e.activation(lg[:], lg[:], AF.Exp, bias=nmx[:, 0:1]), reads=[lg, nmx], writes=[lg])
                P.op("dve", lambda e: e.tensor_tensor(lg[:], lg[:], msk[:], ALU.mult), reads=[lg, msk], writes=[lg])
                P.op("dve", lambda e: e.reduce_sum(esum[:], lg[:], AX.X), reads=[lg], writes=[esum])
                P.op("dve", lambda e: e.reciprocal(esum[:], esum[:]), reads=[esum], writes=[esum])
                P.op("dve", lambda e, tt=tt: e.tensor_scalar(comb[:, tt, :], lg[:], esum[:, 0:1], None, ALU.mult), reads=[lg, esum], writes=[comb])
                P.op("pe", lambda e, tt=tt, pb=pb: e.transpose(pb[0:32, 0:128], comb[:, tt, :], ident_f[:]), reads=[comb, ident_f], writes=[pb])
                P.op("act", lambda e, pb=pb: e.copy(combT[:], pb[0:32, 0:128]), reads=[pb], writes=[combT])
                for half in range(2):
                    P.op("pe", lambda e, pb=pb, half=half: e.matmul(pb[:], combT[:], b2s[:, half * 512:(half + 1) * 512], start=True, stop=True),
                         reads=[combT, b2s], writes=[pb])
                    P.op("act", lambda e, tt=tt, pb=pb, half=half: e.copy(acc[:, tt, half * 512:(half + 1) * 512], pb[:]), reads=[pb], writes=[acc])
            for ex in range(moe_nexp):
                wi = ex % 2
                P.dma("pool", lambda e, wi=wi, ex=ex: [e.dma_start(out=w1s[wi][:, k, :], in_=ew1_v[ex, :, k, :]) for k in range(8)],
                      writes=[w1s[wi]], n=8)
                P.dma("pool", lambda e, ex=ex: [e.dma_start(out=w2s[0][:, k, :], in_=ew2_v[ex, :, k, :]) for k in range(8)],
                      writes=[w2s[0]], n=8)
                P.op("dve", lambda e, wi=wi, ex=ex: e.tensor_scalar(b1s[wi][:], b1all[:, ex * 16:ex * 16 + 8], 1.702, None, ALU.mult), reads=[b1all], writes=[b1s[wi]])
                P.op("dve", lambda e, wi=wi, ex=ex: e.tensor_scalar(b1l[wi][:], b1all[:, ex * 16 + 8:ex * 16 + 16], 1.0, None, ALU.add), reads=[b1all], writes=[b1l[wi]])
                for sub in range(NSUB):
                    ts = slice(sub * 512, (sub + 1) * 512)
                    for fc in range(8):
                        j = fc % 2
                        pg, pl = psA[2 * j], psA[2 * j + 1]

                        def mmu(e, wi=wi, fc=fc, pg=pg, pl=pl, ts=ts):
                            for k in range(8):
                                e.matmul(pg[:], w1s[wi][:, k, fc * 128:(fc + 1) * 128], h2T[:, k, ts], start=(k == 0), stop=(k == 7))
                            for k in range(8):
                                r = e.matmul(pl[:], w1s[wi][:, k, D + fc * 128:D + (fc + 1) * 128], h2T[:, k, ts], start=(k == 0), stop=(k == 7))
                            return r
                        P.op("pe", mmu, reads=[w1s[wi], h2T], writes=[pg, pl])
                        P.op("act", lambda e, wi=wi, fc=fc, j=j, pg=pg: e.activation(sg[j][:], pg[:], AF.Sigmoid, bias=b1s[wi][:, fc:fc + 1], scale=1.702),
                             reads=[pg, b1s[wi]], writes=[sg[j]])
                        P.op("dve", lambda e, fc=fc, j=j, pg=pg, ex=ex: e.scalar_tensor_tensor(gg[j][:], pg[:], b1all[:, ex * 16 + fc:ex * 16 + fc + 1], c7[:], ALU.add, ALU.min),
                             reads=[pg, b1all, c7, sg[j]], writes=[gg[j]])
                        P.op("dve", lambda e, j=j: e.scalar_tensor_tensor(gg[j][:], sg[j][:], SIGMAX, gg[j][:], ALU.min, ALU.mult),
                             reads=[sg[j], gg[j]], writes=[gg[j]])
                        P.op("dve", lambda e, wi=wi, fc=fc, j=j, pl=pl: e.scalar_tensor_tensor(ll[j][:], pl[:], b1l[wi][:, fc:fc + 1], c8[:], ALU.add, ALU.min),
                             reads=[pl, b1l[wi], c8], writes=[ll[j]])
                        P.op("dve", lambda e, j=j, fc=fc, sub=sub: e.scalar_tensor_tensor(actT[sub][:, fc, :], ll[j][:], -6.0, gg[j][:], ALU.max, ALU.mult),
                             reads=[ll[j], gg[j]], writes=[actT[sub]])
                for sub in range(NSUB):
                    for t4 in range(4):
                        tt = sub * 4 + t4
                        for half in range(2):
                            pb = psA[4 + half]

                            def mmy(e, sub=sub, t4=t4, half=half, pb=pb):
                                for fc in range(8):
                                    r = e.matmul(pb[:], actT[sub][:, fc, t4 * 128:(t4 + 1) * 128], w2s[0][:, fc, half * 512:(half + 1) * 512],
                                                 start=(fc == 0), stop=(fc == 7))
                                return r
                            P.op("pe", mmy, reads=[actT[sub], w2s[0]], writes=[pb])
                            P.op("dve", lambda e, tt=tt, half=half, pb=pb, ex=ex: e.scalar_tensor_tensor(
                                acc[:, tt, half * 512:(half + 1) * 512], pb[:], comb[:, tt, ex:ex + 1], acc[:, tt, half * 512:(half + 1) * 512], ALU.mult, ALU.add),
                                reads=[pb, comb, acc], writes=[acc])
            P.dma("sp", lambda e: [e.dma_start(out=frow[:], in_=g_fin_d.partition_broadcast(128))], writes=[frow])
            for tt in range(NTT):
                i = tt % 2
                rows = slice(tc * TC + tt * 128, tc * TC + (tt + 1) * 128)
                P.dma("sp", lambda e, i=i, rows=rows: [e.dma_start(out=x1l[i][:], in_=x1_d[rows, :])], reads=[x1_d], writes=[x1l[i]])
                P.op("dve", lambda e, tt=tt, i=i: e.tensor_tensor(tm2[i][:], acc[:, tt, :], modF[:, 2 * D:3 * D], ALU.mult), reads=[acc, modF], writes=[tm2[i]])
                P.op("dve", lambda e, i=i: e.tensor_tensor(fin[i][:], tm2[i][:], x1l[i][:], ALU.add), reads=[tm2[i], x1l[i]], writes=[fin[i]])
                rmsnorm_mod(fin[i], frow, None, ss2[i], rs2[i], tm2[i], fin[i])
                P.dma("sp", lambda e, i=i, rows=rows, b=b: [e.dma_start(out=out_d[b, rows, :], in_=fin[i][:])], reads=[fin[i]], writes=[out_b])
        P.pop()

    if out_b.w is not None:
        P.wait_all("sp", [out_b.w])
    P.emit()
    return nc


def _zero_fill(P, dst, name):
    z = P.sb(name, [128, 512], F32)
    P.op("dve", lambda e: e.memset(z[:], 0.0), writes=[z])
    for t in range(NT):
        P.dma("sp", lambda e, t=t: [e.dma_start(out=dst[t * 128:(t + 1) * 128, :], in_=z[:])], reads=[z], writes=[dst])


def nsa_stage(P, L):
    proj_d, ynsa_d, psA = L["proj_d"], L["ynsa_d"], L["psA"]
    ident_f, ident_bf = L["ident_f"], L["ident_bf"]
    NQT = L["nsa_nqt"]
    sb = P.sb
    P.push()
    psT = [P.ps("psT%d" % i, [128, 8, 128], BF16) for i in range(2)]
    qT = sb("qT", [64, 8, S], BF16)
    kT = sb("kTn", [64, 8, S], BF16)
    Vaug = sb("Vaug", [128, NT, 4, 65], BF16)
    gate = sb("gate", [128, NT, 24], F32)
    onsa = sb("onsa", [128, NT, 512], F32)
    negcmp = sb("negcmp", [127, S], BF16)
    negdiag4 = sb("negdiag4", [128, 512], BF16)
    negfar4 = sb("negfar4", [128, 512], BF16)
    e32 = sb("e32", [32, S], BF16)
    selm1 = sb("selm1", [128, 16, 32], F32)
    selm2 = sb("selm2", [128, 16, 32], F32)
    zer = sb("zer", [128, 512], BF16)
    kcT = [sb("kcT%d" % g, [64, 128], BF16) for g in range(2)]
    rcmp = [sb("rcmp%d" % g, [127, 97], F32) for g in range(2)]
    negselT = [sb("negselT%d" % g, [32, NT, 128], BF16) for g in range(2)]
    P.dma("sp", lambda e: [e.dma_start(out=negcmp[:], in_=L["negcmp_d"][:, :]),
                           e.dma_start(out=negdiag4[:], in_=L["negdiag4_d"][:, :]),
                           e.dma_start(out=negfar4[:], in_=L["negfar4_d"][:, :]),
                           e.dma_start(out=e32[:], in_=L["e32_d"][:, :]),
                           e.dma_start(out=selm1[:], in_=L["selm1_d"][:, :, :]),
                           e.dma_start(out=selm2[:], in_=L["selm2_d"][:, :, :]),
                           e.dma_start(out=rcmp[0][:, 64:97], in_=L["cmpaug_d"][:, :]),
                           e.dma_start(out=rcmp[1][:, 64:97], in_=L["cmpaug_d"][:, :])],
          writes=[negcmp, negdiag4, negfar4, e32, selm1, selm2, rcmp[0], rcmp[1]], n=8)
    P.op("dve", lambda e: e.memset(zer[:], 0.0), writes=[zer])
    P.op("dve", lambda e: e.memset(Vaug[:].rearrange("p t k d -> p (t k) d")[:, :, 64:65], 1.0), writes=[Vaug])

    P.push()
    qk = [sb("qk%d" % i, [128, 1280], F32) for i in range(2)]
    qkb = [sb("qkb%d" % i, [128, 1280], BF16) for i in range(2)]
    for t in range(NT):
        i = t % 2
        rows = slice(t * 128, (t + 1) * 128)
        P.dma("sp", lambda e, i=i, rows=rows, t=t: [e.dma_start(out=qk[i][:], in_=proj_d[rows, 0:1280]),
                                                  e.dma_start(out=gate[:, t, :], in_=proj_d[rows, O_GATE:O_GATE + 24])],
              reads=[proj_d], writes=[qk[i], gate], n=2)
        P.op("dve", lambda e, i=i: e.tensor_copy(qkb[i][:], qk[i][:]), reads=[qk[i]], writes=[qkb[i]])

        def trq(e, i=i):
            for h in range(8):
                r = e.transpose(psT[0][0:64, h, :], qkb[i][:, h * 64:(h + 1) * 64], ident_bf[:])
            return r
        P.op("pe", trq, reads=[qkb[i], ident_bf], writes=[psT[0]])
        P.op("act", lambda e, t=t: e.copy(qT[:, :, t * 128:(t + 1) * 128], psT[0][0:64, :, :]), reads=[psT[0]], writes=[qT])

        def trk(e, i=i):
            for blk, kind in enumerate((0, 1, 2, 4)):
                for g in range(2):
                    c0 = 512 + kind * 128 + g * 64
                    r = e.transpose(psT[1][0:64, blk * 2 + g, :], qkb[i][:, c0:c0 + 64], ident_bf[:])
            return r
        P.op("pe", trk, reads=[qkb[i], ident_bf], writes=[psT[1]])
        P.op("dve", lambda e, t=t: e.tensor_copy(kT[:, :, t * 128:(t + 1) * 128], psT[1][0:64, :, :]), reads=[psT[1]], writes=[kT])
        P.op("act", lambda e, i=i, t=t: e.copy(Vaug[:, t, 0:2, 0:64], qk[i][:, 512 + 3 * 128:512 + 4 * 128].rearrange("p (g d) -> p g d", g=2)),
             reads=[qk[i]], writes=[Vaug])
        P.op("act", lambda e, i=i, t=t: e.copy(Vaug[:, t, 2:4, 0:64], qk[i][:, 512 + 5 * 128:512 + 6 * 128].rearrange("p (g d) -> p g d", g=2)),
             reads=[qk[i]], writes=[Vaug])
    P.op("act", lambda e: e.activation(gate[:], gate[:], AF.Sigmoid), reads=[gate], writes=[gate])
    P.pop()

    P.push()
    w1c = [sb("w1c%d" % kv, [64, 32, 256], BF16) for kv in range(2)]
    w2c = [sb("w2c%d" % kv, [128, 2, 64], BF16) for kv in range(2)]
    pe_f = sb("pe_f", [32, 128], F32)
    peT = sb("peT", [64, 2, 32], BF16)
    b1col = sb("b1col", [128, 2, 2], F32)
    b1eff = sb("b1eff", [128, 2, 2], F32)
    hidT = sb("hidT", [128, 2, 128], BF16)
    for kv in range(2):
        P.dma("pool", lambda e, kv=kv: [e.dma_start(out=w1c[kv][:], in_=L["cw1_d"][kv].rearrange("(p d) h -> d p h", d=64)),
                                        e.dma_start(out=w2c[kv][:], in_=L["cw2_d"][kv].rearrange("(c p) d -> p c d", p=128))],
              writes=[w1c[kv], w2c[kv]], n=2)
    P.dma("sp", lambda e: [e.dma_start(out=pe_f[:, 0:64], in_=L["pek_d"][:, :]), e.dma_start(out=pe_f[:, 64:128], in_=L["pev_d"][:, :]),
                           e.dma_start(out=b1col[:], in_=L["cb1_d"].rearrange("k (c p) -> p k c", p=128), allow_slow_non_contiguous=True)],
          writes=[pe_f, b1col], n=3)
    for kv in range(2):
        P.op("pe", lambda e, kv=kv: e.transpose(psA[4][0:64, 0:32], pe_f[:, kv * 64:(kv + 1) * 64], ident_f[0:32, 0:32]), reads=[pe_f, ident_f], writes=[psA[4]])
        P.op("dve", lambda e, kv=kv: e.tensor_copy(peT[:, kv, :], psA[4][0:64, 0:32]), reads=[psA[4]], writes=[peT])
        for c in range(2):
            def mb(e, kv=kv, c=c):
                for p in range(32):
                    r = e.matmul(psA[4][:, 0:1], w1c[kv][:, p, c * 128:(c + 1) * 128], peT[:, kv, p:p + 1], start=(p == 0), stop=(p == 31))
                return r
            P.op("pe", mb, reads=[w1c[kv], peT], writes=[psA[4]])
            P.op("dve", lambda e, kv=kv, c=c: e.tensor_tensor(b1eff[:, kv, c:c + 1], psA[4][:, 0:1], b1col[:, kv, c:c + 1], ALU.add),
                 reads=[psA[4], b1col], writes=[b1eff])
        for g in range(2):
            src_blk = (0 if kv == 0 else 2) + g
            for c in range(2):
                def mh(e, kv=kv, c=c, src_blk=src_blk):
                    for p in range(32):
                        r = e.matmul(psA[4][:, 0:127], w1c[kv][:, p, c * 128:(c + 1) * 128], kT[:, src_blk, p:p + 16 * 126 + 1:16],
                                     start=(p == 0), stop=(p == 31))
                    return r
                P.op("pe", mh, reads=[w1c[kv], kT], writes=[psA[4]])
                P.op("act", lambda e, kv=kv, c=c: e.activation(hidT[:, c, 0:127], psA[4][:, 0:127], AF.Silu, bias=b1eff[:, kv, c:c + 1]),
                     reads=[psA[4], b1eff], writes=[hidT])
            if kv == 0:
                def mk(e, g=g):
                    for c in range(2):
                        r = e.matmul(psA[5][0:64, 0:127], w2c[0][:, c, :], hidT[:, c, 0:127], start=(c == 0), stop=(c == 1))
                    return r
                P.op("pe", mk, reads=[w2c[0], hidT], writes=[psA[5]])
                P.op("dve", lambda e, g=g: e.tensor_copy(kcT[g][:, 0:127], psA[5][0:64, 0:127]), reads=[psA[5]], writes=[kcT[g]])
            else:
                def mv(e, g=g):
                    for c in range(2):
                        r = e.matmul(psA[5][0:127, 0:64], hidT[:, c, 0:127], w2c[1][:, c, :], start=(c == 0), stop=(c == 1))
                    return r
                P.op("pe", mv, reads=[w2c[1], hidT], writes=[psA[5]])
                P.op("dve", lambda e, g=g: e.tensor_copy(rcmp[g][:, 0:64], psA[5][0:127, 0:64]), reads=[psA[5]], writes=[rcmp[g]])
    P.pop()

    Ecmp = [sb("Ecmp%d" % h, [127, 512], F32) for h in range(4)]
    PTt = [sb("PTt%d" % i, [128, 512], BF16) for i in range(3)]
    den = sb("den", [128, 4], F32)
    wgt = sb("wgt", [128, 4], F32)
    tmo = sb("tmo", [128, 4, 64], F32)
    timp = sb("timp", [128, 4, 32], F32)
    imp = sb("imp", [128, 32], F32)
    sc1 = sb("sc1", [128, 32], F32)
    sc2 = sb("sc2", [128, 32], F32)
    m8a = sb("m8a", [128, 8], F32)
    m8b = sb("m8b", [128, 8], F32)
    selb = sb("selb", [128, 32], BF16)

    def post(po, g, qt, br, first, width):
        pv = po[:, 0:4 * width].rearrange("p (h w) -> p h w", h=4)
        P.op("dve", lambda e: e.tensor_scalar(den[:], pv[:, :, 64], 1.0e-30, None, ALU.max), reads=[po], writes=[den])
        P.op("dve", lambda e: e.reciprocal(den[:], den[:]), reads=[den], writes=[den])
        gv = gate[:, qt, :].rearrange("p (h b) -> p h b", b=3)[:, 4 * g:4 * g + 4, br]
        P.op("dve", lambda e: e.tensor_tensor(wgt[:], den[:], gv, ALU.mult), reads=[den, gate], writes=[wgt])
        ov = onsa[:, qt, g * 256:(g + 1) * 256].rearrange("p (h d) -> p h d", h=4)
        wb = wgt[:, 0:4].unsqueeze(2).to_broadcast([128, 4, 64])
        if first:
            P.op("dve", lambda e: e.tensor_tensor(ov, pv[:, :, 0:64], wb, ALU.mult), reads=[po, wgt], writes=[onsa])
        else:
            P.op("dve", lambda e: e.tensor_tensor(tmo[:], pv[:, :, 0:64], wb, ALU.mult), reads=[po, wgt], writes=[tmo])
            P.op("dve", lambda e: e.tensor_tensor(ov, ov, tmo[:], ALU.add), reads=[onsa, tmo], writes=[onsa])

    for g in range(2):
        for qb in range((NQT + 3) // 4):
            qs = slice(qb * 512, (qb + 1) * 512)
            for hh in range(4):
                h = 4 * g + hh
                pb = psA[hh % 2]

                def ms(e, pb=pb, h=h, qs=qs, g=g):
                    e.matmul(pb[0:127, :], ident_bf[0:127, 0:127], negcmp[:, qs], start=True, stop=False)
                    return e.matmul(pb[0:127, :], kcT[g][:, 0:127], qT[:, h, qs], start=False, stop=True)
                P.op("pe", ms, reads=[ident_bf, negcmp, kcT[g], qT], writes=[pb])
                P.op("act", lambda e, pb=pb, hh=hh: e.activation(Ecmp[hh][:], pb[0:127, :], AF.Exp, scale=0.125), reads=[pb], writes=[Ecmp[hh]])
            for q4 in range(4):
                qt = qb * 4 + q4
                if qt >= NQT:
                    break
                po = psA[4]

                def mc(e, po=po, q4=q4, g=g):
                    for hh in range(4):
                        r = e.matmul(po[:, hh * 97:(hh + 1) * 97], Ecmp[hh][:, q4 * 128:(q4 + 1) * 128], rcmp[g][:, :], start=True, stop=True)
                    return r
                P.op("pe", mc, reads=Ecmp + [rcmp[g]], writes=[po])
                post(po, g, qt, 0, True, 97)
                if qt >= 8:
                    pv = po[:, 0:388].rearrange("p (h w) -> p h w", h=4)
                    P.op("dve", lambda e, pv=pv: e.tensor_tensor(timp[:], pv[:, :, 65:97], den[:, 0:4].unsqueeze(2).to_broadcast([128, 4, 32]), ALU.mult),
                         reads=[po, den], writes=[timp])
                    P.op("dve", lambda e: e.reduce_sum(imp[:], timp[:].rearrange("p h j -> p j h"), AX.X), reads=[timp], writes=[imp])
                    P.op("dve", lambda e, qt=qt: e.tensor_tensor(sc1[:], imp[:], selm1[:, qt, :], ALU.mult), reads=[imp, selm1], writes=[sc1])
                    P.op("dve", lambda e, qt=qt: e.tensor_tensor(sc1[:], sc1[:], selm2[:, qt, :], ALU.add), reads=[sc1, selm2], writes=[sc1])
                    P.op("dve", lambda e: e.max(m8a[:], sc1[:]), reads=[sc1], writes=[m8a])
                    P.op("dve", lambda e: e.match_replace(sc2[:], m8a[:], sc1[:], -3.0e38), reads=[sc1, m8a], writes=[sc2])
                    P.op("dve", lambda e: e.max(m8b[:], sc2[:]), reads=[sc2], writes=[m8b])
                    P.op("dve", lambda e: e.tensor_scalar(sc2[:], sc1[:], m8b[:, 7:8], None, ALU.is_ge), reads=[sc1, m8b], writes=[sc2])
                    P.op("dve", lambda e: e.tensor_scalar(selb[:], sc2[:], -NEG, NEG, ALU.mult, ALU.add), reads=[sc2], writes=[selb])
                    P.op("pe", lambda e: e.transpose(psT[0][0:32, 0, :], selb[:], ident_bf[:]), reads=[selb, ident_bf], writes=[psT[0]])
                    P.op("act", lambda e, qt=qt, g=g: e.copy(negselT[g][:, qt, :], psT[0][0:32, 0, :]), reads=[psT[0]], writes=[negselT[g]])
        items = []
        for qt in range(NQT):
            for br, kblk, vidx in ((1, 4 + g, g), (2, 6 + g, 2 + g)):
                kts = list(range(0, qt + 1)) if br == 1 else list(range(max(0, qt - 4), qt + 1))
                for ki, kt in enumerate(kts):
                    items.append((qt, br, kblk, vidx, kt, ki, len(kts)))
        sbank = [psA[0], psA[1], psA[5]]

        def emit_scores(n, g=g):
            qt, br, kblk, vidx, kt, ki, nk = items[n]
            qsl = slice(qt * 128, (qt + 1) * 128)
            ksl = slice(kt * 128, (kt + 1) * 128)
            pb = sbank[n % 3]
            ptb = PTt[n % 3]
            cmask = None
            if kt == qt:
                cmask = negdiag4
            elif br == 2 and kt == qt - 4:
                cmask = negfar4
            smask = (br == 1 and qt >= 8)
            rd = [kT, qT]
            if cmask is not None:
                rd.append(cmask)
            if smask:
                rd += [e32, negselT[g]]

            def msc(e):
                started = False
                if cmask is not None:
                    e.matmul(pb[:], ident_bf[:], cmask[:], start=True, stop=False)
                    started = True
                elif smask:
                    e.matmul(pb[:], ident_bf[:], zer[:], start=True, stop=False)
                    started = True
                for hh in range(4):
                    cs = slice(hh * 128, (hh + 1) * 128)
                    if smask:
                        e.matmul(pb[:, cs], e32[:, ksl], negselT[g][:, qt, :], start=False, stop=False)
                    r = e.matmul(pb[:, cs], kT[:, kblk, ksl], qT[:, 4 * g + hh, qsl], start=(not started),
                                 stop=((not started) or hh == 3))
                return r
            P.op("pe", msc, reads=rd + [ident_bf, zer], writes=[pb])
            P.op("act", lambda e: e.activation(ptb[:], pb[:], AF.Exp, scale=0.125), reads=[pb], writes=[ptb])

        def emit_pv(n, g=g):
            qt, br, kblk, vidx, kt, ki, nk = items[n]
            po = psA[2 + (br - 1)]
            ptb = PTt[n % 3]
            if ki == 0:
                P.op("pe", lambda e: e.matmul(po[:, 0:260], ident_bf[:], zer[:, 0:260], start=True, stop=False),
                     reads=[ident_bf, zer], writes=[po])
            last = (ki == nk - 1)

            def mpv(e):
                for hh in range(4):
                    r = e.matmul(po[:, hh * 65:(hh + 1) * 65], ptb[:, hh * 128:(hh + 1) * 128], Vaug[:, kt, vidx, :], start=False,
                                 stop=(last and hh == 3))
                return r
            P.op("pe", mpv, reads=[ptb, Vaug], writes=[po])
            if last:
                post(po, g, qt, br, False, 65)

        LOOK = 2
        for n in range(min(LOOK, len(items))):
            emit_scores(n)
        for n in range(len(items)):
            if n + LOOK < len(items):
                emit_scores(n + LOOK)
            emit_pv(n)
    for qt in range(NQT):
        P.dma("sp", lambda e, qt=qt: [e.dma_start(out=ynsa_d[qt * 128:(qt + 1) * 128, :], in_=onsa[:, qt, :])], reads=[onsa], writes=[ynsa_d])
    P.pop()


def dn_stage(P, L):
    proj_d, ydn_d, psA = L["proj_d"], L["ydn_d"], L["psA"]
    ident_f, ones_f = L["ident_f"], L["ones_f"]
    NCH = L["dn_nchunk"]
    C = 64
    P.push()
    sb = P.sb
    psX = [P.ps("psX%d" % i, [128, 512], F32) for i in range(2)]
    banks = [psA[0:3], psA[3:6], [psX[0], psX[1], psX[0]]]
    tri = sb("tri", [C, C], F32)
    maskpos = sb("maskpos", [C, C], F32)
    strict8 = sb("strict8", [C, 512], F32)
    eye8 = sb("eye8", [C, 512], F32)
    alog = sb("alog", [C, 8], F32)
    dtb = sb("dtb", [C, 8], F32)
    grow = sb("grow", [C, 64], F32)
    P.dma("sp", lambda e: [e.dma_start(out=tri[:], in_=L["tri64_d"][:, :]),
                           e.dma_start(out=maskpos[:], in_=L["maskpos_d"][:, :]),
                           e.dma_start(out=strict8[:], in_=L["strict8_d"][:, :]),
                           e.dma_start(out=eye8[:], in_=L["eye8_d"][:, :]),
                           e.dma_start(out=alog[:], in_=L["alog_d"].partition_broadcast(C)),
                           e.dma_start(out=dtb[:], in_=L["dtb_d"].partition_broadcast(C)),
                           e.dma_start(out=grow[:], in_=L["dng_d"].partition_broadcast(C))],
          writes=[tri, maskpos, strict8, eye8, alog, dtb, grow], n=7)
    nea = sb("nea", [C, 8], F32)
    P.op("act", lambda e: e.activation(nea[:], alog[:], AF.Exp), reads=[alog], writes=[nea])
    P.op("dve", lambda e: e.tensor_scalar(nea[:], nea[:], -1.0, None, ALU.mult), reads=[nea], writes=[nea])
    Sst = sb("Sst", [C, 512], F32)
    P.op("dve", lambda e: e.memset(Sst[:], 0.0), writes=[Sst])
    tmpc = sb("tmpc", [C, 1024], F32)

    names512 = ["Gbc", "Dm", "Ds", "kT", "qT", "Nm0", "Nm1", "Mm0", "Mm1", "Rm", "attn", "attnT", "Xv", "Xk", "qd", "kd",
                "qdT", "uu", "wT", "vn", "gz", "osq", "oo"]
    names8 = ["beta", "nbeta", "ggd", "egc", "edec", "egl", "begc", "oss", "ors"]
    sets = []
    NSET = 3
    for si in range(NSET):
        T = {}
        T["yc"] = sb("yc_%d" % si, [C, 1536], F32)
        T["sm"] = sb("sm_%d" % si, [C, 16 + 512], F32)
        T["ssq"] = sb("ssq_%d" % si, [C, 16], F32)
        T["rq"] = sb("rq_%d" % si, [C, 16], F32)
        T["gcl"] = sb("gcl_%d" % si, [C, 16], F32)
        for nm in names8:
            T[nm] = sb("%s_%d" % (nm, si), [C, 8], F32)
        bf_names = ("kT", "qT", "Nm0", "Nm1", "Mm0", "Mm1", "Rm", "Xv", "Xk")
        for nm in names512:
            T[nm] = sb("%s_%d" % (nm, si), [C, 512], BF16 if nm in bf_names else F32)
        T["Nf"] = sb("Nf_%d" % si, [C, 512], F32)
        T["ps"] = banks[si]
        sets.append(T)

    def v3(buf, lo=0):
        return buf[:, lo:lo + 512].rearrange("p (h d) -> p h d", h=8)

    def bc(buf, lo=0):
        return buf[:, lo:lo + 8].unsqueeze(2).to_broadcast([C, 8, 64])

    def mm8(pb, lhs_fn, rhs_fn, reads, start=True, stop=True, n=64):
        def f(e):
            for h in range(8):
                r = e.matmul(pb[0:C, h * n:(h + 1) * n], lhs_fn(h), rhs_fn(h), start=start, stop=stop)
            return r
        P.op("pe", f, reads=reads, writes=[pb])

    def tr8(pb, src, reads):
        def f(e):
            for h in range(8):
                r = e.transpose(pb[0:C, h * 64:(h + 1) * 64], src[:, h * 64:(h + 1) * 64], ident_f[0:C, 0:C])
            return r
        P.op("pe", f, reads=reads + [ident_f], writes=[pb])

    def hs(buf):
        return lambda h: buf[:, h * 64:(h + 1) * 64]

    def chunk(c, T):
        t0 = c * C
        yc, sm, ssq, rq, gcl = T["yc"], T["sm"], T["ssq"], T["rq"], T["gcl"]
        beta, nbeta, gg, egc, edec, egl, begc, oss, ors = [T[n] for n in names8]
        (Gbc, Dm, Ds, kT, qT, Nm0, Nm1, Mm0, Mm1, Rm, attn, attnT, Xv, Xk, qd, kd, qdT, uu, wT, vn, gz, osq, oo) = [T[n] for n in names512]
        Nm, Mm = [Nm0, Nm1], [Mm0, Mm1]
        Nf = T["Nf"]
        ps = lambda i: T["ps"][i % 3]

        P.dma("sp", lambda e: [e.dma_start(out=yc[:], in_=proj_d[t0:t0 + C, O_DNQKV:O_DNQKV + 1536]),
                               e.dma_start(out=sm[:], in_=proj_d[t0:t0 + C, O_BETA:O_BETA + 528])],
              reads=[proj_d], writes=[yc, sm], n=2)
        yield
        P.op("act", lambda e: e.activation(yc[:], yc[:], AF.Silu), reads=[yc], writes=[yc])
        P.op("act", lambda e: e.activation(gz[:], sm[:, 16:528], AF.Silu), reads=[sm], writes=[gz])
        P.op("act", lambda e: e.activation(beta[:], sm[:, 0:8], AF.Sigmoid), reads=[sm], writes=[beta])
        yield
        P.op("dve", lambda e: e.tensor_tensor(tmpc[:, 0:1024], yc[:, 0:1024], yc[:, 0:1024], ALU.mult), reads=[yc], writes=[tmpc])
        P.op("dve", lambda e: e.reduce_sum(ssq[:], tmpc[:, 0:1024].rearrange("p (h d) -> p h d", h=16), AX.X), reads=[tmpc], writes=[ssq])
        yield
        P.op("dve", lambda e: e.tensor_scalar(ssq[:], ssq[:], EPS, None, ALU.add), reads=[ssq], writes=[ssq])
        P.op("act", lambda e: e.activation(rq[:], ssq[:], AF.Ln), reads=[ssq], writes=[rq])
        P.op("act", lambda e: e.activation(rq[:], rq[:], AF.Exp, scale=-0.5), reads=[rq], writes=[rq])
        yield
        P.op("dve", lambda e: e.tensor_scalar(rq[:, 0:8], rq[:, 0:8], 0.125, None, ALU.mult), reads=[rq], writes=[rq])
        P.op("dve", lambda e: e.tensor_tensor(yc[:, 0:1024].rearrange("p (h d) -> p h d", h=16), yc[:, 0:1024].rearrange("p (h d) -> p h d", h=16),
                                              rq[:, 0:16].unsqueeze(2).to_broadcast([C, 16, 64]), ALU.mult), reads=[yc, rq], writes=[yc])
        yield
        P.op("dve", lambda e: e.tensor_scalar(nbeta[:], beta[:], -1.0, None, ALU.mult), reads=[beta], writes=[nbeta])
        P.op("dve", lambda e: e.tensor_tensor(gg[:], sm[:, 8:16], dtb[:], ALU.add), reads=[sm, dtb], writes=[gg])
        P.op("act", lambda e: e.activation(gg[:], gg[:], AF.Exp), reads=[gg], writes=[gg])
        P.op("act", lambda e: e.activation(gg[:], gg[:], AF.Ln, bias=ones_f[0:C, 0:1]), reads=[gg, ones_f], writes=[gg])
        P.op("dve", lambda e: e.tensor_tensor(gg[:], gg[:], nea[:], ALU.mult), reads=[gg, nea], writes=[gg])
        yield
        pb = ps(0)

        def mg(e):
            e.matmul(pb[0:C, 0:8], tri[:], gg[:], start=True, stop=True)
            return e.matmul(pb[0:C, 8:16], ones_f[0:C, 0:C], gg[:], start=True, stop=True)
        P.op("pe", mg, reads=[tri, gg, ones_f], writes=[pb])
        P.op("dve", lambda e: e.tensor_copy(gcl[:], pb[0:C, 0:16]), reads=[pb], writes=[gcl])
        yield
        P.op("act", lambda e: e.activation(egc[:], gcl[:, 0:8], AF.Exp), reads=[gcl], writes=[egc])
        P.op("act", lambda e: e.activation(egl[:], gcl[:, 8:16], AF.Exp), reads=[gcl], writes=[egl])
        P.op("dve", lambda e: e.tensor_tensor(edec[:], gcl[:, 8:16], gcl[:, 0:8], ALU.subtract), reads=[gcl], writes=[edec])
        P.op("act", lambda e: e.activation(edec[:], edec[:], AF.Exp), reads=[edec], writes=[edec])
        P.op("dve", lambda e: e.tensor_tensor(begc[:], beta[:], egc[:], ALU.mult), reads=[beta, egc], writes=[begc])
        yield
        P.op("dve", lambda e: e.tensor_copy(v3(Gbc), bc(gg)), reads=[gg], writes=[Gbc])
        pd = ps(1)

        def md(e):
            for h in range(8):
                e.matmul(pd[0:C, h * 64:(h + 1) * 64], Gbc[:, h * 64:(h + 1) * 64], tri[:], start=True, stop=False)
                r = e.matmul(pd[0:C, h * 64:(h + 1) * 64], ident_f[0:C, 0:C], maskpos[:], start=False, stop=True)
            return r
        P.op("pe", md, reads=[Gbc, tri, ident_f, maskpos], writes=[pd])
        yield
        P.op("dve", lambda e: e.tensor_tensor(v3(Dm), pd[0:C, :].rearrange("p (h d) -> p h d", h=8), bc(gcl), ALU.subtract),
             reads=[pd, gcl], writes=[Dm])
        P.op("act", lambda e: e.activation(Dm[:], Dm[:], AF.Exp, scale=-1.0), reads=[Dm], writes=[Dm])
        P.op("dve", lambda e: e.tensor_tensor(Ds[:], Dm[:], strict8[:], ALU.mult), reads=[Dm, strict8], writes=[Ds])
        yield
        tr8(ps(2), yc[:, 512:1024], [yc])
        P.op("act", lambda e: e.copy(kT[:], ps(2)[0:C, :]), reads=[ps(2)], writes=[kT])
        yield
        tr8(ps(3), yc[:, 0:512], [yc])
        P.op("dve", lambda e: e.tensor_copy(qT[:], ps(3)[0:C, :]), reads=[ps(3)], writes=[qT])
        yield
        mm8(ps(4), hs(kT), hs(kT), [kT])
        P.op("dve", lambda e: e.tensor_tensor(v3(Nf), ps(4)[0:C, :].rearrange("p (h d) -> p h d", h=8), bc(nbeta), ALU.mult),
             reads=[ps(4), nbeta], writes=[Nf])
        P.op("dve", lambda e: e.tensor_tensor(Nf[:], Nf[:], Ds[:], ALU.mult), reads=[Nf, Ds], writes=[Nf])
        P.op("act", lambda e: e.copy(Nm[0][:], Nf[:]), reads=[Nf], writes=[Nm[0]])
        yield
        mm8(ps(5), hs(qT), hs(kT), [qT, kT])
        P.op("dve", lambda e: e.tensor_tensor(attn[:], ps(5)[0:C, :], Dm[:], ALU.mult), reads=[ps(5), Dm], writes=[attn])
        yield
        P.op("dve", lambda e: e.tensor_tensor(v3(Xv), v3(yc, 1024), bc(beta), ALU.mult), reads=[yc, beta], writes=[Xv])
        P.op("dve", lambda e: e.tensor_tensor(v3(Xk), v3(yc, 512), bc(begc), ALU.mult), reads=[yc, begc], writes=[Xk])
        yield
        P.op("dve", lambda e: e.tensor_tensor(v3(qd), v3(yc, 0), bc(egc), ALU.mult), reads=[yc, egc], writes=[qd])
        P.op("dve", lambda e: e.tensor_tensor(v3(kd), v3(yc, 512), bc(edec), ALU.mult), reads=[yc, edec], writes=[kd])
        yield
        tr8(ps(0), Nf, [Nf])
        P.op("act", lambda e: e.copy(Mm[0][:], ps(0)[0:C, :]), reads=[ps(0)], writes=[Mm[0]])
        P.op("dve", lambda e: e.tensor_tensor(Rm[:], ps(0)[0:C, :], eye8[:], ALU.add), reads=[ps(0), eye8], writes=[Rm])
        yield
        for k in range(1, 6):
            a, b_ = (k - 1) % 2, k % 2
            if k < 5:
                mm8(ps(1), hs(Nm[a]), hs(Mm[a]), [Nm[a], Mm[a]])
            mm8(ps(2), hs(Mm[a]), hs(Nm[a]), [Nm[a], Mm[a]])
            yield
            if k < 5:
                P.op("act", lambda e, b_=b_: e.copy(Mm[b_][:], ps(1)[0:C, :]), reads=[ps(1)], writes=[Mm[b_]])
            P.op("dve", lambda e, b_=b_: e.tensor_copy(Nm[b_][:], ps(2)[0:C, :]), reads=[ps(2)], writes=[Nm[b_]])
            yield
            mm8(ps(3), hs(Nm[b_]), hs(Rm), [Nm[b_], Rm])
            P.op("dve", lambda e: e.tensor_tensor(Rm[:], Rm[:], ps(3)[0:C, :], ALU.add), reads=[Rm, ps(3)], writes=[Rm])
            yield
        mm8(ps(4), hs(Rm), hs(Xv), [Rm, Xv])
        P.op("act", lambda e: e.copy(uu[:], ps(4)[0:C, :]), reads=[ps(4)], writes=[uu])
        yield
        mm8(ps(5), hs(Xk), hs(Rm), [Rm, Xk])
        P.op("dve", lambda e: e.tensor_copy(wT[:], ps(5)[0:C, :]), reads=[ps(5)], writes=[wT])
        yield
        tr8(ps(0), attn, [attn])
        P.op("act", lambda e: e.copy(attnT[:], ps(0)[0:C, :]), reads=[ps(0)], writes=[attnT])
        yield
        tr8(ps(1), qd, [qd])
        P.op("dve", lambda e: e.tensor_copy(qdT[:], ps(1)[0:C, :]), reads=[ps(1)], writes=[qdT])
        P.op("dve", lambda e: e.tensor_tensor(v3(gz), v3(gz), grow[:, 0:64].unsqueeze(1).to_broadcast([C, 8, 64]), ALU.mult), reads=[gz, grow], writes=[gz])
        yield
        mm8(ps(2), hs(wT), hs(Sst), [wT, Sst])
        P.op("dve", lambda e: e.tensor_tensor(vn[:], uu[:], ps(2)[0:C, :], ALU.subtract), reads=[uu, ps(2)], writes=[vn])
        yield
        po = ps(3)

        def mo(e):
            for h in range(8):
                e.matmul(po[0:C, h * 64:(h + 1) * 64], qdT[:, h * 64:(h + 1) * 64], Sst[:, h * 64:(h + 1) * 64], start=True, stop=False)
                r = e.matmul(po[0:C, h * 64:(h + 1) * 64], attnT[:, h * 64:(h + 1) * 64], vn[:, h * 64:(h + 1) * 64], start=False, stop=True)
            return r
        P.op("pe", mo, reads=[qdT, Sst, attnT, vn], writes=[po])
        mm8(ps(4), hs(kd), hs(vn), [kd, vn])
        yield
        P.op("dve", lambda e: e.tensor_tensor(v3(Sst), v3(Sst), bc(egl), ALU.mult), reads=[Sst, egl], writes=[Sst])
        P.op("dve", lambda e: e.tensor_tensor(Sst[:], Sst[:], ps(4)[0:C, :], ALU.add), reads=[Sst, ps(4)], writes=[Sst])
        yield
        P.op("dve", lambda e: e.tensor_copy(oo[:], po[0:C, :]), reads=[po], writes=[oo])
        P.op("dve", lambda e: e.tensor_tensor(osq[:], oo[:], oo[:], ALU.mult), reads=[oo], writes=[osq])
        P.op("dve", lambda e: e.reduce_sum(oss[:], v3(osq), AX.X), reads=[osq], writes=[oss])
        yield
        P.op("dve", lambda e: e.tensor_scalar(oss[:], oss[:], 1.0 / 64, EPS, ALU.mult, ALU.add), reads=[oss], writes=[oss])
        P.op("act", lambda e: e.activation(ors[:], oss[:], AF.Ln), reads=[oss], writes=[ors])
        P.op("act", lambda e: e.activation(ors[:], ors[:], AF.Exp, scale=-0.5), reads=[ors], writes=[ors])
        yield
        P.op("dve", lambda e: e.tensor_tensor(v3(oo), v3(oo), bc(ors), ALU.mult), reads=[oo, ors], writes=[oo])
        P.op("dve", lambda e: e.tensor_tensor(oo[:], oo[:], gz[:], ALU.mult), reads=[oo, gz], writes=[oo])
        P.dma("sp", lambda e: [e.dma_start(out=ydn_d[t0:t0 + C, :], in_=oo[:])], reads=[oo], writes=[ydn_d])
        yield

    gens = {}
    nxt = 0
    STAG = 10
    for si in range(NSET):
        if nxt < NCH:
            gens[si] = chunk(nxt, sets[si])
            nxt += 1
            if si < NSET - 1:
                for _ in range(STAG):
                    for sj in list(gens.keys()):
                        try:
                            next(gens[sj])
                        except StopIteration:
                            del gens[sj]
    while gens:
        for si in list(gens.keys()):
            try:
                next(gens[si])
            except StopIteration:
                del gens[si]
                if nxt < NCH:
                    gens[si] = chunk(nxt, sets[si])
                    nxt += 1
    P.pop()


def core_inputs(inputs, core, nseq=BPC):
    m = {}
    b0 = core * nseq
    g = lambda k: np.ascontiguousarray(np.asarray(inputs[k], dtype=np.float32))
    m["x"] = np.ascontiguousarray(inputs["x"][b0:b0 + nseq])
    m["c"] = np.ascontiguousarray(inputs["c"][b0:b0 + nseq])
    m["w_ada"] = g("w_ada")[0]
    m["b_ada"] = g("b_ada")[0].reshape(1, -1)
    m["g_norm_mix"] = g("g_norm_mix")[0].reshape(1, -1)
    m["g_norm_ffn"] = g("g_norm_ffn")[0].reshape(1, -1)
    m["final_norm_g"] = g("final_norm_g").reshape(1, -1)
    m["w_in"] = g("w_in")[0]
    m["w_branch"] = g("w_branch")[0]
    m["w_out"] = g("w_out")[0]
    m["router_w"] = g("router_w")[0]
    m["router_b"] = g("router_b")[0].reshape(1, -1)
    m["exp_w1"] = g("exp_w1")[0]
    m["exp_b1"] = g("exp_b1")[0]
    m["exp_w2"] = g("exp_w2")[0]
    m["exp_b2"] = g("exp_b2")[0]
    m["cmp_pe_k"] = g("cmp_pe_k")[0]
    m["cmp_pe_v"] = g("cmp_pe_v")[0]
    m["cmp_w1"] = g("cmp_w1")[0]
    m["cmp_b1"] = g("cmp_b1")[0]
    m["cmp_w2"] = g("cmp_w2")[0]
    m["dn_conv_w"] = g("dn_conv_w")[0].reshape(1, -1)
    m["dn_a_log"] = g("dn_a_log")[0].reshape(1, -1)
    m["dn_dt_bias"] = g("dn_dt_bias")[0].reshape(1, -1)
    m["dn_norm_g"] = g("dn_norm_g")[0].reshape(1, -1)
    m.update(host_consts())
    return m


def kernel(**inputs):
    nc = build()
    in_maps = [core_inputs(inputs, c) for c in range(NCORES)]
    res = run_bass_kernel_spmd(nc, in_maps, core_ids=list(range(NCORES)))
    return np.concatenate([r["out"] for r in res.results], axis=0).astype(np.float32)
```

```python
from contextlib import ExitStack
import numpy as np
import ml_dtypes
import concourse.bass as bass
import concourse.mybir as mybir
from concourse.bass_utils import run_bass_kernel_spmd

F32 = mybir.dt.float32
BF16 = mybir.dt.bfloat16
AF = mybir.ActivationFunctionType
ALU = mybir.AluOpType
AX = mybir.AxisListType

D = 1024
S = 2048
NT = S // 128
INW = 5416
NCORES = 8
BPC = 4
EPS = 1e-6
NEG = -30000.0
O_Q, O_KV, O_GATE, O_DNQKV, O_BETA, O_A, O_Z, O_MERGE = 0, 512, 1280, 1304, 2840, 2848, 2856, 3368


class Buf:
    def __init__(self, t, name):
        self.t = t
        self.name = name
        self.w = None
        self.r = {}
        self.dsem = None
        self.psum = False

    def __getitem__(self, k):
        return self.t[k]


class Prog:
    ENGS = ("pe", "dve", "act", "pool", "sp")

    def __init__(self):
        self.nc = bass.Bass("TRN2", target_bir_lowering=False)
        self.es = ExitStack()
        self.semstack = ExitStack()
        self.sems = {}
        self.semval = {}
        self.ops = {e: [] for e in self.ENGS}
        self.seen = {e: {} for e in self.ENGS}
        for e in self.ENGS:
            self._newsem("E_" + e)
        self.nbuf = 0
        self.stack = []
        self.uid = 0

    def _newsem(self, key):
        if key in self.sems:
            return key
        self.sems[key] = self.semstack.enter_context(self.nc.semaphore(key))
        self.semval[key] = 0
        return key

    def sb(self, name, shape, dt):
        self.uid += 1
        t = self.es.enter_context(self.nc.sbuf_tensor("s%d_%s" % (self.uid, name), list(shape), dt))
        return Buf(t, name)

    def ps(self, name, shape, dt):
        self.uid += 1
        t = self.es.enter_context(self.nc.psum_tensor("p%d_%s" % (self.uid, name), list(shape), dt))
        b = Buf(t, name)
        b.psum = True
        return b

    def dram(self, name, shape, dt, kind="Internal"):
        t = self.nc.dram_tensor(name, list(shape), dt, kind=kind).ap()
        return Buf(t, name)

    def _deps(self, eng, reads, writes):
        own = "E_" + eng
        deps = {}

        def add(tok):
            if tok is None:
                return
            k, v = tok
            if deps.get(k, 0) < v:
                deps[k] = v

        for b in reads:
            if b.w is not None:
                if b.w[0] == own and eng == "pe":
                    continue
                add(b.w)
            if b.psum:
                for k, v in b.r.items():
                    if k != own:
                        add((k, v))
        for b in writes:
            if b.w is not None and (b.w[0] != own or eng != "pe"):
                add(b.w)
            for k, v in b.r.items():
                if k != own or eng != "pe":
                    add((k, v))
        waits = []
        seen = self.seen[eng]
        for k, v in deps.items():
            if seen.get(k, 0) < v:
                waits.append((k, v))
                seen[k] = v
        return waits

    def _commit(self, tok, reads, writes):
        k, v = tok
        for b in reads:
            if b.r.get(k, 0) < v:
                b.r[k] = v
        for b in writes:
            b.w = tok
            b.r = {}

    def op(self, eng, fn, reads=(), writes=()):
        waits = self._deps(eng, reads, writes)
        own = "E_" + eng
        self.semval[own] += 1
        tok = (own, self.semval[own])
        self.seen[eng][own] = max(self.seen[eng].get(own, 0), 0)
        self._emit1(eng, waits, fn, [(own, 1)], False)
        self._commit(tok, reads, writes)

    def dma(self, eng, fn, reads=(), writes=(), n=1, sem=None):
        waits = self._deps(eng, reads, writes)
        if sem is None:
            tgt = writes[0]
            if tgt.dsem is None:
                tgt.dsem = self._newsem("D_" + tgt.name)
            sem = tgt.dsem
        self.semval[sem] += 16 * n
        tok = (sem, self.semval[sem])
        self._emit1(eng, waits, fn, [(sem, 16)] * n, True)
        self._commit(tok, reads, writes)
        return tok

    def wait_all(self, eng, toks):
        waits = [(k, v) for (k, v) in toks]
        self._emit1(eng, waits, None, [], False)

    def _emit1(self, eng, waits, fn, incs, isdma):
        nc = self.nc
        e = {"pe": nc.tensor, "dve": nc.vector, "act": nc.scalar, "pool": nc.gpsimd, "sp": nc.sync}[eng]
        sems = self.sems
        for k, v in waits:
            e.wait_ge(sems[k], v)
        if fn is None:
            return
        r = fn(e)
        if isdma:
            assert len(r) == len(incs), (len(r), len(incs))
            for ins, (k, a) in zip(r, incs):
                ins.then_inc(sems[k], a)
        else:
            r.then_inc(sems[incs[0][0]], 1)

    def barrier(self):
        for eng in self.ENGS:
            waits = []
            for k, v in self.semval.items():
                if v > 0 and k != "E_" + eng and self.seen[eng].get(k, 0) < v:
                    waits.append((k, v))
                    self.seen[eng][k] = v
            self._emit1(eng, waits, None, [], False)

    def push(self):
        self.stack.append(self.es)
        self.es = ExitStack()

    def pop(self):
        self.barrier()
        self.es.close()
        self.es = self.stack.pop()

    def emit(self):
        self.es.close()
        self.semstack.close()
        return

    def emit_old(self):
        nc = self.nc
        sems = self.sems
        with nc.Block() as block:
            def mk(eng):
                lst = self.ops[eng]

                def body(e):
                    for waits, fn, incs, isdma in lst:
                        for k, v in waits:
                            e.wait_ge(sems[k], v)
                        if fn is None:
                            continue
                        r = fn(e)
                        if isdma:
                            assert len(r) == len(incs), (len(r), len(incs))
                            for ins, (k, a) in zip(r, incs):
                                ins.then_inc(sems[k], a)
                        else:
                            r.then_inc(sems[incs[0][0]], 1)
                return body
            block.tensor(mk("pe"))
            block.vector(mk("dve"))
            block.scalar(mk("act"))
            block.gpsimd(mk("pool"))
            block.sync(mk("sp"))
        self.es.close()


def bf(x):
    return np.asarray(x, dtype=np.float32).astype(ml_dtypes.bfloat16)


SIGMAX = float(1.0 / (1.0 + np.exp(-1.702 * 7.0)))


def host_consts():
    c = {}
    c["ident_bf"] = bf(np.eye(128))
    c["ident_f"] = np.eye(128, dtype=np.float32)
    c["ones_f"] = np.ones((128, 128), np.float32)
    i = np.arange(64)
    tri = (i[:, None] <= i[None, :]).astype(np.float32)
    c["tri64"] = tri
    c["maskpos"] = np.where(i[:, None] >= i[None, :], 0.0, 1.0e4).astype(np.float32)
    strict = (i[:, None] > i[None, :]).astype(np.float32)
    c["strict8"] = np.ascontiguousarray(np.broadcast_to(strict[:, None, :], (64, 8, 64))).reshape(64, 512)
    t = np.arange(S)
    n = np.arange(127)
    c["negcmp"] = bf(np.where((16 * n[:, None] + 31) <= t[None, :], 0.0, NEG))
    kk = np.arange(128)
    diag = np.where(kk[:, None] <= kk[None, :], 0.0, NEG)
    far = np.where(kk[:, None] > kk[None, :], 0.0, NEG)
    c["negdiag4"] = bf(np.tile(diag, (1, 4)))
    c["negfar4"] = bf(np.tile(far, (1, 4)))
    j = np.arange(32)
    c["e32"] = bf((t[None, :] // 64 == j[:, None]).astype(np.float32))
    cur = t // 64
    forced = (j[None, :] == 0) | (j[None, :] == cur[:, None]) | (j[None, :] == cur[:, None] - 1)
    causal = (64 * j[None, :]) <= t[:, None]
    m1 = np.where(forced, 0.0, np.where(causal, 1.0, 0.0)).astype(np.float32)
    m2 = np.where(forced, 1.0e9, np.where(causal, 0.0, -1.0e30)).astype(np.float32)
    c["selm1"] = np.ascontiguousarray(m1.reshape(16, 128, 32).transpose(1, 0, 2))
    c["selm2"] = np.ascontiguousarray(m2.reshape(16, 128, 32).transpose(1, 0, 2))
    ov = np.clip(np.minimum(16 * n[:, None] + 32, 64 * j[None, :] + 64) - np.maximum(16 * n[:, None], 64 * j[None, :]), 0, None) / 32.0
    cmpaug = np.zeros((127, 33), np.float32)
    cmpaug[:, 0] = 1.0
    cmpaug[:, 1:33] = ov
    c["cmpaug"] = cmpaug
    c["eye8"] = np.ascontiguousarray(np.broadcast_to(np.eye(64, dtype=np.float32)[:, None, :], (64, 8, 64))).reshape(64, 512)
    return c


def build(nseq=BPC, dbg=None, inject=False, moe_nexp=32, moe_ntc=None, moe_cut=9, inject_nsa=False, inject_dn=False, dn_nchunk=32, nsa_nqt=16):
    P = Prog()
    nc = P.nc
    IN = lambda name, shape, dt=F32: nc.dram_tensor(name, list(shape), dt, kind="ExternalInput").ap()
    x_d = IN("x", [nseq, S, D])
    c_d = IN("c", [nseq, D])
    w_ada_d = IN("w_ada", [D, 6 * D])
    b_ada_d = IN("b_ada", [1, 6 * D])
    g_mix_d = IN("g_norm_mix", [1, D])
    g_ffn_d = IN("g_norm_ffn", [1, D])
    g_fin_d = IN("final_norm_g", [1, D])
    w_in_d = IN("w_in", [D, INW])
    w_br_d = IN("w_branch", [2, 512, D])
    w_out_d = IN("w_out", [D, D])
    rw_d = IN("router_w", [D, 32])
    rb_d = IN("router_b", [1, 32])
    ew1_d = IN("exp_w1", [32, D, 2 * D])
    eb1_d = IN("exp_b1", [32, 2 * D])
    ew2_d = IN("exp_w2", [32, D, D])
    eb2_d = IN("exp_b2", [32, D])
    convw_d = IN("dn_conv_w", [1, 4 * 1536])
    alog_d = IN("dn_a_log", [1, 8])
    dtb_d = IN("dn_dt_bias", [1, 8])
    dng_d = IN("dn_norm_g", [1, 64])
    pek_d = IN("cmp_pe_k", [32, 64])
    pev_d = IN("cmp_pe_v", [32, 64])
    cw1_d = IN("cmp_w1", [2, 2048, 256])
    cb1_d = IN("cmp_b1", [2, 256])
    cw2_d = IN("cmp_w2", [2, 256, 64])
    negcmp_d = IN("negcmp", [127, S], BF16)
    negdiag4_d = IN("negdiag4", [128, 512], BF16)
    negfar4_d = IN("negfar4", [128, 512], BF16)
    e32_d = IN("e32", [32, S], BF16)
    selm1_d = IN("selm1", [128, 16, 32])
    selm2_d = IN("selm2", [128, 16, 32])
    cmpaug_d = IN("cmpaug", [127, 33])
    tri64_d = IN("tri64", [64, 64])
    maskpos_d = IN("maskpos", [64, 64])
    strict8_d = IN("strict8", [64, 512])
    eye8_d = IN("eye8", [64, 512])
    ident_bf_d = IN("ident_bf", [128, 128], BF16)
    ident_f_d = IN("ident_f", [128, 128])
    ones_f_d = IN("ones_f", [128, 128])
    inject_nsa = inject_nsa or inject
    inject_dn = inject_dn or inject
    if inject_nsa:
        ynsa_in = IN("ynsa_in", [S, 512])
    if inject_dn:
        ydn_in = IN("ydn_in", [S, 512])
    out_d = nc.dram_tensor("out", [nseq, S, D], F32, kind="ExternalOutput").ap()
    dbg_d = None
    if dbg == "proj":
        dbg_d = nc.dram_tensor("dbg", [S, INW], F32, kind="ExternalOutput").ap()
    elif dbg == "x1":
        dbg_d = nc.dram_tensor("dbg", [S, D], F32, kind="ExternalOutput").ap()
    elif dbg in ("y_dn", "y_nsa"):
        dbg_d = nc.dram_tensor("dbg", [S, 512], F32, kind="ExternalOutput").ap()

    ident_bf = P.sb("ident_bf", [128, 128], BF16)
    ident_f = P.sb("ident_f", [128, 128], F32)
    ones_f = P.sb("ones_f", [128, 128], F32)
    cT = P.sb("cT", [128, nseq, 8], F32)
    sc = P.sb("sc", [128, nseq, 8], F32)
    modF = P.sb("modF", [128, 3 * D], F32)

    def ld_consts(e):
        r = []
        r.append(e.dma_start(out=ident_bf[:], in_=ident_bf_d[:, :]))
        r.append(e.dma_start(out=ident_f[:], in_=ident_f_d[:, :]))
        r.append(e.dma_start(out=ones_f[:], in_=ones_f_d[:, :]))
        for b in range(nseq):
            r.append(e.dma_start(out=cT[:, b, :], in_=c_d[b].rearrange("(k p) -> p k", p=128),
                                 allow_slow_non_contiguous=True))
        return r
    P.dma("sp", ld_consts, writes=[ident_bf, ident_f, ones_f, cT], n=3 + nseq)
    P.op("act", lambda e: e.activation(sc[:], cT[:], AF.Silu), reads=[cT], writes=[sc])

    psA = [P.ps("psA%d" % i, [128, 512], F32) for i in range(6)]
    proj_d = P.dram("proj_scr", [S, INW], F32)
    ynsa_d = P.dram("ynsa_scr", [S, 512], F32)
    ydn_d = P.dram("ydn_scr", [S, 512], F32)
    x1_d = P.dram("x1_scr", [S, D], F32)
    if inject_nsa:
        ynsa_d = Buf(ynsa_in, "ynsa_in")
    if inject_dn:
        ydn_d = Buf(ydn_in, "ydn_in")
    out_b = Buf(out_d, "outd")

    w_ada_v = w_ada_d.rearrange("(k p) n -> p k n", p=128)
    w_in_v = w_in_d.rearrange("(k p) n -> p k n", p=128)

    def rmsnorm_mod(src, grow_a, shift_ap, ssb, rsb, tmp, dst, shift_buf=None):
        P.op("act", lambda e: e.activation(tmp[:], src[:], AF.Square, accum_out=ssb[:]), reads=[src], writes=[tmp, ssb])
        P.op("dve", lambda e: e.tensor_scalar(rsb[:], ssb[:], 1.0 / D, EPS, ALU.mult, ALU.add), reads=[ssb], writes=[rsb])
        P.op("act", lambda e: e.activation(ssb[:], rsb[:], AF.Sqrt), reads=[rsb], writes=[ssb])
        P.op("dve", lambda e: e.reciprocal(rsb[:], ssb[:]), reads=[ssb], writes=[rsb])
        P.op("dve", lambda e: e.scalar_tensor_tensor(tmp[:], src[:], rsb[:, 0:1], grow_a[:], ALU.mult, ALU.mult),
             reads=[src, rsb, grow_a], writes=[tmp])
        if shift_ap is None:
            P.op("dve", lambda e: e.tensor_copy(dst[:], tmp[:]), reads=[tmp], writes=[dst])
        else:
            P.op("dve", lambda e: e.tensor_tensor(dst[:], tmp[:], shift_ap(), ALU.add), reads=[tmp, shift_buf], writes=[dst])

    def transpose8(src_bf, ptb, dst_ap_fn, dst_buf, eng="act"):
        def tr(e):
            for k in range(8):
                r = e.transpose(ptb[:, k, :], src_bf[:, k * 128:(k + 1) * 128], ident_bf[:])
            return r
        P.op("pe", tr, reads=[src_bf, ident_bf], writes=[ptb])
        if eng == "act":
            P.op("act", lambda e: e.copy(dst_ap_fn(), ptb[:]), reads=[ptb], writes=[dst_buf])
        else:
            P.op("dve", lambda e: e.tensor_copy(dst_ap_fn(), ptb[:]), reads=[ptb], writes=[dst_buf])

    mod_d = P.dram("mod_scr", [nseq, 6 * D], F32)
    P.push()
    wada = [P.sb("wada%d" % i, [128, 8, 256], F32) for i in range(2)]
    b_ada_sb = [P.sb("b_ada_sb%d" % i, [1, 256], F32) for i in range(2)]
    mstg = [P.sb("mstg%d" % i, [nseq, 256], F32) for i in range(2)]
    for j in range(24):
        wb_ = wada[j % 2]
        bb_ = b_ada_sb[j % 2]
        P.dma("sp", lambda e, j=j, wb_=wb_: [e.dma_start(out=wb_[:], in_=w_ada_v[:, :, j * 256:(j + 1) * 256])], writes=[wb_])
        P.dma("sp", lambda e, j=j, bb_=bb_: [e.dma_start(out=bb_[:], in_=b_ada_d[:, j * 256:(j + 1) * 256])], writes=[bb_])
        pb = psA[j % 2]

        def mm0(e, wb_=wb_, pb=pb, bb_=bb_):
            for k in range(8):
                e.matmul(pb[0:nseq, 0:256], sc[:, :, k], wb_[:, k, :], start=(k == 0), stop=False)
            return e.matmul(pb[0:nseq, 0:256], ones_f[0:1, 0:nseq], bb_[0:1, :], start=False, stop=True)
        P.op("pe", mm0, reads=[sc, wb_, ones_f, bb_], writes=[pb])
        P.op("act", lambda e, j=j, pb=pb: e.copy(mstg[j % 2][:], pb[0:nseq, 0:256]), reads=[pb], writes=[mstg[j % 2]])
        P.dma("sp", lambda e, j=j: [e.dma_start(out=mod_d[:, j * 256:(j + 1) * 256], in_=mstg[j % 2][:])], reads=[mstg[j % 2]], writes=[mod_d])
    P.pop()

    for b in range(nseq):
        P.push()
        modA = P.sb("modA", [128, 3 * D], F32)
        P.push()
        psT = [P.ps("psT%d" % i, [128, 8, 128], BF16) for i in range(2)]
        g_mix_row = P.sb("g_mix_row", [128, D], F32)
        P.dma("sp", lambda e: [e.dma_start(out=g_mix_row[:], in_=g_mix_d.partition_broadcast(128))], writes=[g_mix_row])
        arow = P.sb("arow", [128, D], F32)
        hT = P.sb("hT", [128, 8, 4 + S], BF16)
        P.op("dve", lambda e: e.memset(hT[:, :, 0:4], 0.0), writes=[hT])
        cwrow = P.sb("cwrow", [128, 4 * 1536], F32)
        P.dma("sp", lambda e: [e.dma_start(out=cwrow[:], in_=convw_d.partition_broadcast(128))], writes=[cwrow])
        wtap = [P.sb("wtap%d" % j, [128, 8, 512], BF16) for j in range(4)]
        xt = [P.sb("xt%d" % i, [128, D], F32) for i in range(2)]
        ss = [P.sb("ss%d" % i, [128, 1], F32) for i in range(2)]
        rstd = [P.sb("rstd%d" % i, [128, 1], F32) for i in range(2)]
        xm = [P.sb("xm%d" % i, [128, D], F32) for i in range(2)]
        hb = [P.sb("hb%d" % i, [128, D], BF16) for i in range(2)]
        wblk = [P.sb("wblk%d" % i, [128, 8, 512], BF16) for i in range(2)]
        pst = [P.sb("pst%d" % i, [128, 512], F32) for i in range(3)]

        P.dma("sp", lambda e, b=b: [e.dma_start(out=modA[:], in_=mod_d[b:b + 1, 0:3 * D].partition_broadcast(128)),
                                    e.dma_start(out=modF[:], in_=mod_d[b:b + 1, 3 * D:6 * D].partition_broadcast(128))],
              reads=[mod_d], writes=[modA, modF], n=2)
        P.op("dve", lambda e: e.scalar_tensor_tensor(arow[:], modA[:, D:2 * D], 1.0, g_mix_row[:], ALU.add, ALU.mult),
             reads=[modA, g_mix_row], writes=[arow])

        for t in range(NT):
            i = t % 2
            P.dma("sp", lambda e, b=b, t=t, i=i: [e.dma_start(out=xt[i][:], in_=x_d[b, t * 128:(t + 1) * 128, :])], writes=[xt[i]])
            rmsnorm_mod(xt[i], arow, lambda: modA[:, 0:D], ss[i], rstd[i], xm[i], hb[i], modA)
            transpose8(hb[i], psT[i], lambda t=t: hT[:, :, 4 + t * 128:4 + (t + 1) * 128], hT)

        blocks = [(0, 512, False), (512, 512, False), (1024, O_DNQKV - 1024, False)]
        blocks += [(O_DNQKV + i * 512, 512, True) for i in range(3)]
        c0 = O_BETA
        while c0 < INW:
            blocks.append((c0, min(512, INW - c0), False))
            c0 += 512
        cnt = 0
        for j, (c0, cw, isdn) in enumerate(blocks):
            wb_ = wblk[j % 2]
            P.dma("pool", lambda e, wb_=wb_, c0=c0, cw=cw: [e.dma_start(out=wb_[:, :, 0:cw], in_=w_in_v[:, :, c0:c0 + cw])], writes=[wb_])
            if isdn:
                for tp in range(4):
                    cc = tp * 1536 + (c0 - O_DNQKV)
                    P.op("dve", lambda e, tp=tp, wb_=wb_, cc=cc: e.tensor_tensor(
                        wtap[tp][:], wb_[:], cwrow[:, cc:cc + 512].unsqueeze(1).to_broadcast([128, 8, 512]), ALU.mult),
                        reads=[wb_, cwrow], writes=[wtap[tp]])
            for t in range(NT):
                pb = psA[cnt % 4]
                sbt = pst[cnt % 3]
                cnt += 1
                if isdn:
                    def mm(e, t=t, pb=pb):
                        n = 0
                        for tp in range(4):
                            for k in range(8):
                                lo = 4 + t * 128 - 3 + tp
                                r = e.matmul(pb[:, 0:512], hT[:, k, lo:lo + 128], wtap[tp][:, k, :], start=(n == 0), stop=(n == 31))
                                n += 1
                        return r
                    P.op("pe", mm, reads=[hT] + wtap, writes=[pb])
                else:
                    def mm(e, t=t, wb_=wb_, pb=pb, cw=cw):
                        for k in range(8):
                            r = e.matmul(pb[:, 0:cw], hT[:, k, 4 + t * 128:4 + (t + 1) * 128], wb_[:, k, 0:cw], start=(k == 0), stop=(k == 7))
                        return r
                    P.op("pe", mm, reads=[hT, wb_], writes=[pb])
                if cnt % 2 == 0:
                    P.op("act", lambda e, pb=pb, sbt=sbt, cw=cw: e.copy(sbt[:, 0:cw], pb[:, 0:cw]), reads=[pb], writes=[sbt])
                else:
                    P.op("dve", lambda e, pb=pb, sbt=sbt, cw=cw: e.tensor_copy(sbt[:, 0:cw], pb[:, 0:cw]), reads=[pb], writes=[sbt])
                P.dma("sp", lambda e, t=t, sbt=sbt, c0=c0, cw=cw: [e.dma_start(out=proj_d[t * 128:(t + 1) * 128, c0:c0 + cw], in_=sbt[:, 0:cw])],
                      reads=[sbt], writes=[proj_d])
        P.pop()

        if not inject_dn:
            dn_stage(P, locals())
            if dbg == "y_dn":
                otok = P.dma("sp", lambda e: [e.dma_start(out=dbg_d[:, :], in_=ydn_d[:, :])], reads=[ydn_d], writes=[], sem=P._newsem("D_dbg"))
                P.wait_all("sp", [otok])
                break
        if not inject_nsa:
            nsa_stage(P, locals())
            if dbg == "y_nsa":
                otok = P.dma("sp", lambda e: [e.dma_start(out=dbg_d[:, :], in_=ynsa_d[:, :])], reads=[ynsa_d], writes=[], sem=P._newsem("D_dbg"))
                P.wait_all("sp", [otok])
                break

        P.push()
        psT = [P.ps("psT%d" % i, [128, 8, 128], BF16) for i in range(2)]
        wbr = P.sb("wbr", [128, 8, D], BF16)
        wout = P.sb("wout", [128, 8, D], BF16)
        P.dma("pool", lambda e: [e.dma_start(out=wbr[:, 0:4, :], in_=w_br_d[0].rearrange("(k p) n -> p k n", p=128)),
                                 e.dma_start(out=wbr[:, 4:8, :], in_=w_br_d[1].rearrange("(k p) n -> p k n", p=128))],
              writes=[wbr], n=2)
        P.dma("pool", lambda e: [e.dma_start(out=wout[:], in_=w_out_d.rearrange("(k p) n -> p k n", p=128))], writes=[wout])
        yin = [P.sb("yin%d" % i, [128, D], F32) for i in range(2)]
        ybf = [P.sb("ybf%d" % i, [128, D], BF16) for i in range(2)]
        yT = [P.sb("yT%d" % i, [128, 8, 128], BF16) for i in range(2)]
        mlog = [P.sb("mlog%d" % i, [128, 2 * D], F32) for i in range(2)]
        mix = [P.sb("mix%d" % i, [128, D], F32) for i in range(2)]
        mixb = [P.sb("mixb%d" % i, [128, D], BF16) for i in range(2)]
        mT = [P.sb("mT%d" % i, [128, 8, 128], BF16) for i in range(2)]
        xr = [P.sb("xr%d" % i, [128, D], F32) for i in range(2)]
        x1t = [P.sb("x1t%d" % i, [128, D], F32) for i in range(2)]
        tmpm = P.sb("tmpm", [128, 512], F32)
        for t in range(NT):
            i = t % 2
            rows = slice(t * 128, (t + 1) * 128)
            P.dma("sp", lambda e, i=i, rows=rows: [e.dma_start(out=yin[i][:, 0:512], in_=ynsa_d[rows, :]),
                                                    e.dma_start(out=yin[i][:, 512:1024], in_=ydn_d[rows, :])],
                  reads=[ynsa_d, ydn_d], writes=[yin[i]], n=2)
            P.dma("sp", lambda e, i=i, rows=rows: [e.dma_start(out=mlog[i][:], in_=proj_d[rows, O_MERGE:O_MERGE + 2 * D])],
                  reads=[proj_d], writes=[mlog[i]])
            P.dma("sp", lambda e, i=i, rows=rows, b=b: [e.dma_start(out=xr[i][:], in_=x_d[b, rows, :])], writes=[xr[i]])
            P.op("dve", lambda e, i=i: e.tensor_copy(ybf[i][:], yin[i][:]), reads=[yin[i]], writes=[ybf[i]])
            transpose8(ybf[i], psT[i], lambda i=i: yT[i][:], yT[i], eng="dve")
            P.op("act", lambda e, i=i: e.activation(mlog[i][:], mlog[i][:], AF.Sigmoid), reads=[mlog[i]], writes=[mlog[i]])
            for half in range(2):
                cs = slice(half * 512, (half + 1) * 512)
                for br in range(2):
                    pb = psA[half * 2 + br]

                    def mm(e, i=i, br=br, pb=pb, cs=cs):
                        for k in range(4):
                            r = e.matmul(pb[:], yT[i][:, br * 4 + k, :], wbr[:, br * 4 + k, cs], start=(k == 0), stop=(k == 3))
                        return r
                    P.op("pe", mm, reads=[yT[i], wbr], writes=[pb])
                P.op("dve", lambda e, i=i, cs=cs, half=half: e.tensor_tensor(mix[i][:, cs], psA[half * 2][:], mlog[i][:, cs], ALU.mult),
                     reads=[psA[half * 2], mlog[i]], writes=[mix[i]])
                P.op("dve", lambda e, i=i, half=half: e.tensor_tensor(tmpm[:], psA[half * 2 + 1][:], mlog[i][:, D + half * 512:D + (half + 1) * 512], ALU.mult),
                     reads=[psA[half * 2 + 1], mlog[i]], writes=[tmpm])
                P.op("dve", lambda e, i=i, cs=cs: e.tensor_tensor(mixb[i][:, cs], mix[i][:, cs], tmpm[:], ALU.add),
                     reads=[mix[i], tmpm], writes=[mixb[i]])
            transpose8(mixb[i], psT[i], lambda i=i: mT[i][:], mT[i], eng="act")
            for half in range(2):
                cs = slice(half * 512, (half + 1) * 512)
                pb = psA[4 + half]

                def mm(e, i=i, pb=pb, cs=cs):
                    for k in range(8):
                        r = e.matmul(pb[:], mT[i][:, k, :], wout[:, k, cs], start=(k == 0), stop=(k == 7))
                    return r
                P.op("pe", mm, reads=[mT[i], wout], writes=[pb])
                P.op("dve", lambda e, i=i, pb=pb, cs=cs, half=half: e.tensor_tensor(
                    x1t[i][:, cs], pb[:], modA[:, 2 * D + half * 512:2 * D + (half + 1) * 512], ALU.mult),
                    reads=[pb, modA], writes=[x1t[i]])
            P.op("dve", lambda e, i=i: e.tensor_tensor(x1t[i][:], x1t[i][:], xr[i][:], ALU.add), reads=[x1t[i], xr[i]], writes=[x1t[i]])
            P.dma("sp", lambda e, i=i, rows=rows: [e.dma_start(out=x1_d[rows, :], in_=x1t[i][:])], reads=[x1t[i]], writes=[x1_d])
        P.pop()
        P.pop()
        if dbg == "x1":
            otok = P.dma("sp", lambda e: [e.dma_start(out=dbg_d[:, :], in_=x1_d[:, :])], reads=[x1_d], writes=[], sem=P._newsem("D_dbg"))
            P.wait_all("sp", [otok])
            break

        P.push()
        psT = [P.ps("psT%d" % i, [128, 8, 128], BF16) for i in range(2)]
        frow = P.sb("frow", [128, D], F32)
        rb_row = P.sb("rb_row", [128, 32], F32)
        rw = P.sb("rw", [128, 8, 32], BF16)
        P.dma("sp", lambda e: [e.dma_start(out=rb_row[:], in_=rb_d.partition_broadcast(128))], writes=[rb_row])
        P.dma("pool", lambda e: [e.dma_start(out=rw[:], in_=rw_d.rearrange("(k p) n -> p k n", p=128))], writes=[rw])
        TC = 1024
        NTC = S // TC
        NTT = TC // 128
        NSUB = TC // 512
        acc = P.sb("acc", [128, NTT, D], F32)
        h2T = P.sb("h2T", [128, 8, TC], BF16)
        x1l = [P.sb("x1l%d" % i, [128, D], F32) for i in range(2)]
        ss2 = [P.sb("ssb%d" % i, [128, 1], F32) for i in range(2)]
        rs2 = [P.sb("rsb%d" % i, [128, 1], F32) for i in range(2)]
        tm2 = [P.sb("tm2%d" % i, [128, D], F32) for i in range(2)]
        h2b = [P.sb("h2b%d" % i, [128, D], BF16) for i in range(2)]
        comb = P.sb("comb", [128, NTT, 32], F32)
        lg = P.sb("lg", [128, 32], F32)
        m8 = P.sb("m8", [128, 8], F32)
        nmx = P.sb("nmx", [128, 1], F32)
        msk = P.sb("msk", [128, 32], F32)
        esum = P.sb("esum", [128, 1], F32)
        combT = P.sb("combT", [32, 128], F32)
        w1s = [P.sb("w1s%d" % i, [128, 8, 2 * D], BF16) for i in range(2)]
        w2s = [P.sb("w2s0", [128, 8, D], BF16)] * 2
        b1all = P.sb("b1all", [128, 512], F32)
        b1raw = P.sb("b1raw", [128, 128], F32)
        eb1_v = eb1_d.rearrange("e (c p) -> (e c) p", p=128)
        for q in range(4):
            P.dma("sp", lambda e, q=q: [e.dma_start(out=b1raw[:], in_=eb1_v[q * 128:(q + 1) * 128, :])], writes=[b1raw])
            P.op("pe", lambda e: e.transpose(psA[0][:, 0:128], b1raw[:], ident_f[:]), reads=[b1raw, ident_f], writes=[psA[0]])
            P.op("act", lambda e, q=q: e.copy(b1all[:, q * 128:(q + 1) * 128], psA[0][:, 0:128]), reads=[psA[0]], writes=[b1all])
        b1s = [P.sb("b1s%d" % i, [128, 8], F32) for i in range(2)]
        b1l = [P.sb("b1l%d" % i, [128, 8], F32) for i in range(2)]
        b2s = P.sb("b2s", [32, D], F32)
        c7 = P.sb("c7", [128, 512], BF16)
        c8 = P.sb("c8", [128, 512], BF16)
        P.op("dve", lambda e: e.memset(c7[:], 7.0), writes=[c7])
        P.op("dve", lambda e: e.memset(c8[:], 8.0), writes=[c8])
        P.dma("sp", lambda e: [e.dma_start(out=b2s[:], in_=eb2_d[:, :])], writes=[b2s])
        actT = [P.sb("actT%d" % i, [128, 8, 512], BF16) for i in range(2)]
        sg = [P.sb("sg0", [128, 512], F32)] * 2
        gg = [P.sb("gg%d" % i, [128, 512], F32) for i in range(2)]
        ll = [P.sb("ll0", [128, 512], F32)] * 2
        fin = x1l
        ew1_v = ew1_d.rearrange("e (k p) n -> e p k n", p=128)
        ew2_v = ew2_d.rearrange("e (k p) n -> e p k n", p=128)
        for tc in range(NTC if moe_ntc is None else moe_ntc):
            P.dma("sp", lambda e: [e.dma_start(out=frow[:], in_=g_ffn_d.partition_broadcast(128))], writes=[frow])
            P.op("dve", lambda e: e.scalar_tensor_tensor(frow[:], modF[:, D:2 * D], 1.0, frow[:], ALU.add, ALU.mult),
                 reads=[modF, frow], writes=[frow])
            for tt in range(NTT):
                i = tt % 2
                rows = slice(tc * TC + tt * 128, tc * TC + (tt + 1) * 128)
                P.dma("sp", lambda e, i=i, rows=rows: [e.dma_start(out=x1l[i][:], in_=x1_d[rows, :])], reads=[x1_d], writes=[x1l[i]])
                rmsnorm_mod(x1l[i], frow, lambda: modF[:, 0:D], ss2[i], rs2[i], tm2[i], h2b[i], modF)
                transpose8(h2b[i], psT[i], lambda tt=tt: h2T[:, :, tt * 128:(tt + 1) * 128], h2T)
                pb = psA[4 + i]

                def mmr(e, tt=tt, pb=pb):
                    for k in range(8):
                        r = e.matmul(pb[:, 0:32], h2T[:, k, tt * 128:(tt + 1) * 128], rw[:, k, :], start=(k == 0), stop=(k == 7))
                    return r
                P.op("pe", mmr, reads=[h2T, rw], writes=[pb])
                P.op("dve", lambda e, pb=pb: e.tensor_tensor(lg[:], pb[:, 0:32], rb_row[:], ALU.add), reads=[pb, rb_row], writes=[lg])
                P.op("dve", lambda e: e.max(m8[:], lg[:]), reads=[lg], writes=[m8])
                P.op("dve", lambda e: e.tensor_scalar(msk[:], lg[:], m8[:, 3:4], None, ALU.is_ge), reads=[lg, m8], writes=[msk])
                P.op("dve", lambda e: e.tensor_scalar(nmx[:], m8[:, 0:1], -1.0, None, ALU.mult), reads=[m8], writes=[nmx])
                P.op("act", lambda e: e.activation(lg[:], lg[:], AF.Exp, bias=nmx[:, 0:1]), reads=[lg, nmx], writes=[lg])
                P.op("dve", lambda e: e.tensor_tensor(lg[:], lg[:], msk[:], ALU.mult), reads=[lg, msk], writes=[lg])
                P.op("dve", lambda e: e.reduce_sum(esum[:], lg[:], AX.X), reads=[lg], writes=[esum])
                P.op("dve", lambda e: e.reciprocal(esum[:], esum[:]), reads=[esum], writes=[esum])
                P.op("dve", lambda e, tt=tt: e.tensor_scalar(comb[:, tt, :], lg[:], esum[:, 0:1], None, ALU.mult), reads=[lg, esum], writes=[comb])
                P.op("pe", lambda e, tt=tt, pb=pb: e.transpose(pb[0:32, 0:128], comb[:, tt, :], ident_f[:]), reads=[comb, ident_f], writes=[pb])
                P.op("act", lambda e, pb=pb: e.copy(combT[:], pb[0:32, 0:128]), reads=[pb], writes=[combT])
                for half in range(2):
                    P.op("pe", lambda e, pb=pb, half=half: e.matmul(pb[:], combT[:], b2s[:, half * 512:(half + 1) * 512], start=True, stop=True),
                         reads=[combT, b2s], writes=[pb])
                    P.op("act", lambda e, tt=tt, pb=pb, half=half: e.copy(acc[:, tt, half * 512:(half + 1) * 512], pb[:]), reads=[pb], writes=[acc])
            for ex in range(moe_nexp):
                wi = ex % 2
                P.dma("pool", lambda e, wi=wi, ex=ex: [e.dma_start(out=w1s[wi][:, k, :], in_=ew1_v[ex, :, k, :]) for k in range(8)],
                      writes=[w1s[wi]], n=8)
                P.dma("pool", lambda e, ex=ex: [e.dma_start(out=w2s[0][:, k, :], in_=ew2_v[ex, :, k, :]) for k in range(8)],
                      writes=[w2s[0]], n=8)
                P.op("dve", lambda e, wi=wi, ex=ex: e.tensor_scalar(b1s[wi][:], b1all[:, ex * 16:ex * 16 + 8], 1.702, None, ALU.mult), reads=[b1all], writes=[b1s[wi]])
                P.op("dve", lambda e, wi=wi, ex=ex: e.tensor_scalar(b1l[wi][:], b1all[:, ex * 16 + 8:ex * 16 + 16], 1.0, None, ALU.add), reads=[b1all], writes=[b1l[wi]])
                for sub in range(NSUB):
                    ts = slice(sub * 512, (sub + 1) * 512)
                    for fc in range(8):
                        j = fc % 2
                        pg, pl = psA[2 * j], psA[2 * j + 1]

                        def mmu(e, wi=wi, fc=fc, pg=pg, pl=pl, ts=ts):
                            for k in range(8):
                                e.matmul(pg[:], w1s[wi][:, k, fc * 128:(fc + 1) * 128], h2T[:, k, ts], start=(k == 0), stop=(k == 7))
                            for k in range(8):
                                r = e.matmul(pl[:], w1s[wi][:, k, D + fc * 128:D + (fc + 1) * 128], h2T[:, k, ts], start=(k == 0), stop=(k == 7))
                            return r
                        P.op("pe", mmu, reads=[w1s[wi], h2T], writes=[pg, pl])
                        P.op("act", lambda e, wi=wi, fc=fc, j=j, pg=pg: e.activation(sg[j][:], pg[:], AF.Sigmoid, bias=b1s[wi][:, fc:fc + 1], scale=1.702),
                             reads=[pg, b1s[wi]], writes=[sg[j]])
                        P.op("dve", lambda e, fc=fc, j=j, pg=pg, ex=ex: e.scalar_tensor_tensor(gg[j][:], pg[:], b1all[:, ex * 16 + fc:ex * 16 + fc + 1], c7[:], ALU.add, ALU.min),
                             reads=[pg, b1all, c7, sg[j]], writes=[gg[j]])
                        P.op("dve", lambda e, j=j: e.scalar_tensor_tensor(gg[j][:], sg[j][:], SIGMAX, gg[j][:], ALU.min, ALU.mult),
                             reads=[sg[j], gg[j]], writes=[gg[j]])
                        P.op("dve", lambda e, wi=wi, fc=fc, j=j, pl=pl: e.scalar_tensor_tensor(ll[j][:], pl[:], b1l[wi][:, fc:fc + 1], c8[:], ALU.add, ALU.min),
                             reads=[pl, b1l[wi], c8], writes=[ll[j]])
                        P.op("dve", lambda e, j=j, fc=fc, sub=sub: e.scalar_tensor_tensor(actT[sub][:, fc, :], ll[j][:], -6.0, gg[j][:], ALU.max, ALU.mult),
                             reads=[ll[j], gg[j]], writes=[actT[sub]])
                for sub in range(NSUB):
                    for t4 in range(4):
                        tt = sub * 4 + t4
                        for half in range(2):
                            pb = psA[4 + half]

                            def mmy(e, sub=sub, t4=t4, half=half, pb=pb):
                                for fc in range(8):
                                    r = e.matmul(pb[:], actT[sub][:, fc, t4 * 128:(t4 + 1) * 128], w2s[0][:, fc, half * 512:(half + 1) * 512],
                                                 start=(fc == 0), stop=(fc == 7))
                                return r
                            P.op("pe", mmy, reads=[actT[sub], w2s[0]], writes=[pb])
                            P.op("dve", lambda e, tt=tt, half=half, pb=pb, ex=ex: e.scalar_tensor_tensor(
                                acc[:, tt, half * 512:(half + 1) * 512], pb[:], comb[:, tt, ex:ex + 1], acc[:, tt, half * 512:(half + 1) * 512], ALU.mult, ALU.add),
                                reads=[pb, comb, acc], writes=[acc])
            P.dma("sp", lambda e: [e.dma_start(out=frow[:], in_=g_fin_d.partition_broadcast(128))], writes=[frow])
            for tt in range(NTT):
                i = tt % 2
                rows = slice(tc * TC + tt * 128, tc * TC + (tt + 1) * 128)
                P.dma("sp", lambda e, i=i, rows=rows: [e.dma_start(out=x1l[i][:], in_=x1_d[rows, :])], reads=[x1_d], writes=[x1l[i]])
                P.op("dve", lambda e, tt=tt, i=i: e.tensor_tensor(tm2[i][:], acc[:, tt, :], modF[:, 2 * D:3 * D], ALU.mult), reads=[acc, modF], writes=[tm2[i]])
                P.op("dve", lambda e, i=i: e.tensor_tensor(fin[i][:], tm2[i][:], x1l[i][:], ALU.add), reads=[tm2[i], x1l[i]], writes=[fin[i]])
                rmsnorm_mod(fin[i], frow, None, ss2[i], rs2[i], tm2[i], fin[i])
                P.dma("sp", lambda e, i=i, rows=rows, b=b: [e.dma_start(out=out_d[b, rows, :], in_=fin[i][:])], reads=[fin[i]], writes=[out_b])
        P.pop()

    if out_b.w is not None:
        P.wait_all("sp", [out_b.w])
    P.emit()
    return nc


def _zero_fill(P, dst, name):
    z = P.sb(name, [128, 512], F32)
    P.op("dve", lambda e: e.memset(z[:], 0.0), writes=[z])
    for t in range(NT):
        P.dma("sp", lambda e, t=t: [e.dma_start(out=dst[t * 128:(t + 1) * 128, :], in_=z[:])], reads=[z], writes=[dst])


def nsa_stage(P, L):
    proj_d, ynsa_d, psA = L["proj_d"], L["ynsa_d"], L["psA"]
    ident_f, ident_bf = L["ident_f"], L["ident_bf"]
    NQT = L["nsa_nqt"]
    sb = P.sb
    P.push()
    psT = [P.ps("psT%d" % i, [128, 8, 128], BF16) for i in range(2)]
    qT = sb("qT", [64, 8, S], BF16)
    kT = sb("kTn", [64, 8, S], BF16)
    Vaug = sb("Vaug", [128, NT, 4, 65], BF16)
    gate = sb("gate", [128, NT, 24], F32)
    onsa = sb("onsa", [128, NT, 512], F32)
    negcmp = sb("negcmp", [127, S], BF16)
    negdiag4 = sb("negdiag4", [128, 512], BF16)
    negfar4 = sb("negfar4", [128, 512], BF16)
    e32 = sb("e32", [32, S], BF16)
    selm1 = sb("selm1", [128, 16, 32], F32)
    selm2 = sb("selm2", [128, 16, 32], F32)
    zer = sb("zer", [128, 512], BF16)
    kcT = [sb("kcT%d" % g, [64, 128], BF16) for g in range(2)]
    rcmp = [sb("rcmp%d" % g, [127, 97], F32) for g in range(2)]
    negselT = [sb("negselT%d" % g, [32, NT, 128], BF16) for g in range(2)]
    P.dma("sp", lambda e: [e.dma_start(out=negcmp[:], in_=L["negcmp_d"][:, :]),
                           e.dma_start(out=negdiag4[:], in_=L["negdiag4_d"][:, :]),
                           e.dma_start(out=negfar4[:], in_=L["negfar4_d"][:, :]),
                           e.dma_start(out=e32[:], in_=L["e32_d"][:, :]),
                           e.dma_start(out=selm1[:], in_=L["selm1_d"][:, :, :]),
                           e.dma_start(out=selm2[:], in_=L["selm2_d"][:, :, :]),
                           e.dma_start(out=rcmp[0][:, 64:97], in_=L["cmpaug_d"][:, :]),
                           e.dma_start(out=rcmp[1][:, 64:97], in_=L["cmpaug_d"][:, :])],
          writes=[negcmp, negdiag4, negfar4, e32, selm1, selm2, rcmp[0], rcmp[1]], n=8)
    P.op("dve", lambda e: e.memset(zer[:], 0.0), writes=[zer])
    P.op("dve", lambda e: e.memset(Vaug[:].rearrange("p t k d -> p (t k) d")[:, :, 64:65], 1.0), writes=[Vaug])

    P.push()
    qk = [sb("qk%d" % i, [128, 1280], F32) for i in range(2)]
    qkb = [sb("qkb%d" % i, [128, 1280], BF16) for i in range(2)]
    for t in range(NT):
        i = t % 2
        rows = slice(t * 128, (t + 1) * 128)
        P.dma("sp", lambda e, i=i, rows=rows, t=t: [e.dma_start(out=qk[i][:], in_=proj_d[rows, 0:1280]),
                                                  e.dma_start(out=gate[:, t, :], in_=proj_d[rows, O_GATE:O_GATE + 24])],
              reads=[proj_d], writes=[qk[i], gate], n=2)
        P.op("dve", lambda e, i=i: e.tensor_copy(qkb[i][:], qk[i][:]), reads=[qk[i]], writes=[qkb[i]])

        def trq(e, i=i):
            for h in range(8):
                r = e.transpose(psT[0][0:64, h, :], qkb[i][:, h * 64:(h + 1) * 64], ident_bf[:])
            return r
        P.op("pe", trq, reads=[qkb[i], ident_bf], writes=[psT[0]])
        P.op("act", lambda e, t=t: e.copy(qT[:, :, t * 128:(t + 1) * 128], psT[0][0:64, :, :]), reads=[psT[0]], writes=[qT])

        def trk(e, i=i):
            for blk, kind in enumerate((0, 1, 2, 4)):
                for g in range(2):
                    c0 = 512 + kind * 128 + g * 64
                    r = e.transpose(psT[1][0:64, blk * 2 + g, :], qkb[i][:, c0:c0 + 64], ident_bf[:])
            return r
        P.op("pe", trk, reads=[qkb[i], ident_bf], writes=[psT[1]])
        P.op("dve", lambda e, t=t: e.tensor_copy(kT[:, :, t * 128:(t + 1) * 128], psT[1][0:64, :, :]), reads=[psT[1]], writes=[kT])
        P.op("act", lambda e, i=i, t=t: e.copy(Vaug[:, t, 0:2, 0:64], qk[i][:, 512 + 3 * 128:512 + 4 * 128].rearrange("p (g d) -> p g d", g=2)),
             reads=[qk[i]], writes=[Vaug])
        P.op("act", lambda e, i=i, t=t: e.copy(Vaug[:, t, 2:4, 0:64], qk[i][:, 512 + 5 * 128:512 + 6 * 128].rearrange("p (g d) -> p g d", g=2)),
             reads=[qk[i]], writes=[Vaug])
    P.op("act", lambda e: e.activation(gate[:], gate[:], AF.Sigmoid), reads=[gate], writes=[gate])
    P.pop()

    P.push()
    w1c = [sb("w1c%d" % kv, [64, 32, 256], BF16) for kv in range(2)]
    w2c = [sb("w2c%d" % kv, [128, 2, 64], BF16) for kv in range(2)]
    pe_f = sb("pe_f", [32, 128], F32)
    peT = sb("peT", [64, 2, 32], BF16)
    b1col = sb("b1col", [128, 2, 2], F32)
    b1eff = sb("b1eff", [128, 2, 2], F32)
    hidT = sb("hidT", [128, 2, 128], BF16)
    for kv in range(2):
        P.dma("pool", lambda e, kv=kv: [e.dma_start(out=w1c[kv][:], in_=L["cw1_d"][kv].rearrange("(p d) h -> d p h", d=64)),
                                        e.dma_start(out=w2c[kv][:], in_=L["cw2_d"][kv].rearrange("(c p) d -> p c d", p=128))],
              writes=[w1c[kv], w2c[kv]], n=2)
    P.dma("sp", lambda e: [e.dma_start(out=pe_f[:, 0:64], in_=L["pek_d"][:, :]), e.dma_start(out=pe_f[:, 64:128], in_=L["pev_d"][:, :]),
                           e.dma_start(out=b1col[:], in_=L["cb1_d"].rearrange("k (c p) -> p k c", p=128), allow_slow_non_contiguous=True)],
          writes=[pe_f, b1col], n=3)
    for kv in range(2):
        P.op("pe", lambda e, kv=kv: e.transpose(psA[4][0:64, 0:32], pe_f[:, kv * 64:(kv + 1) * 64], ident_f[0:32, 0:32]), reads=[pe_f, ident_f], writes=[psA[4]])
        P.op("dve", lambda e, kv=kv: e.tensor_copy(peT[:, kv, :], psA[4][0:64, 0:32]), reads=[psA[4]], writes=[peT])
        for c in range(2):
            def mb(e, kv=kv, c=c):
                for p in range(32):
                    r = e.matmul(psA[4][:, 0:1], w1c[kv][:, p, c * 128:(c + 1) * 128], peT[:, kv, p:p + 1], start=(p == 0), stop=(p == 31))
                return r
            P.op("pe", mb, reads=[w1c[kv], peT], writes=[psA[4]])
            P.op("dve", lambda e, kv=kv, c=c: e.tensor_tensor(b1eff[:, kv, c:c + 1], psA[4][:, 0:1], b1col[:, kv, c:c + 1], ALU.add),
                 reads=[psA[4], b1col], writes=[b1eff])
        for g in range(2):
            src_blk = (0 if kv == 0 else 2) + g
            for c in range(2):
                def mh(e, kv=kv, c=c, src_blk=src_blk):
                    for p in range(32):
                        r = e.matmul(psA[4][:, 0:127], w1c[kv][:, p, c * 128:(c + 1) * 128], kT[:, src_blk, p:p + 16 * 126 + 1:16],
                                     start=(p == 0), stop=(p == 31))
                    return r
                P.op("pe", mh, reads=[w1c[kv], kT], writes=[psA[4]])
                P.op("act", lambda e, kv=kv, c=c: e.activation(hidT[:, c, 0:127], psA[4][:, 0:127], AF.Silu, bias=b1eff[:, kv, c:c + 1]),
                     reads=[psA[4], b1eff], writes=[hidT])
            if kv == 0:
                def mk(e, g=g):
                    for c in range(2):
                        r = e.matmul(psA[5][0:64, 0:127], w2c[0][:, c, :], hidT[:, c, 0:127], start=(c == 0), stop=(c == 1))
                    return r
                P.op("pe", mk, reads=[w2c[0], hidT], writes=[psA[5]])
                P.op("dve", lambda e, g=g: e.tensor_copy(kcT[g][:, 0:127], psA[5][0:64, 0:127]), reads=[psA[5]], writes=[kcT[g]])
            else:
                def mv(e, g=g):
                    for c in range(2):
                        r = e.matmul(psA[5][0:127, 0:64], hidT[:, c, 0:127], w2c[1][:, c, :], start=(c == 0), stop=(c == 1))
                    return r
                P.op("pe", mv, reads=[w2c[1], hidT], writes=[psA[5]])
                P.op("dve", lambda e, g=g: e.tensor_copy(rcmp[g][:, 0:64], psA[5][0:127, 0:64]), reads=[psA[5]], writes=[rcmp[g]])
    P.pop()

    Ecmp = [sb("Ecmp%d" % h, [127, 512], F32) for h in range(4)]
    PTt = [sb("PTt%d" % i, [128, 512], BF16) for i in range(3)]
    den = sb("den", [128, 4], F32)
    wgt = sb("wgt", [128, 4], F32)
    tmo = sb("tmo", [128, 4, 64], F32)
    timp = sb("timp", [128, 4, 32], F32)
    imp = sb("imp", [128, 32], F32)
    sc1 = sb("sc1", [128, 32], F32)
    sc2 = sb("sc2", [128, 32], F32)
    m8a = sb("m8a", [128, 8], F32)
    m8b = sb("m8b", [128, 8], F32)
    selb = sb("selb", [128, 32], BF16)

    def post(po, g, qt, br, first, width):
        pv = po[:, 0:4 * width].rearrange("p (h w) -> p h w", h=4)
        P.op("dve", lambda e: e.tensor_scalar(den[:], pv[:, :, 64], 1.0e-30, None, ALU.max), reads=[po], writes=[den])
        P.op("dve", lambda e: e.reciprocal(den[:], den[:]), reads=[den], writes=[den])
        gv = gate[:, qt, :].rearrange("p (h b) -> p h b", b=3)[:, 4 * g:4 * g + 4, br]
        P.op("dve", lambda e: e.tensor_tensor(wgt[:], den[:], gv, ALU.mult), reads=[den, gate], writes=[wgt])
        ov = onsa[:, qt, g * 256:(g + 1) * 256].rearrange("p (h d) -> p h d", h=4)
        wb = wgt[:, 0:4].unsqueeze(2).to_broadcast([128, 4, 64])
        if first:
            P.op("dve", lambda e: e.tensor_tensor(ov, pv[:, :, 0:64], wb, ALU.mult), reads=[po, wgt], writes=[onsa])
        else:
            P.op("dve", lambda e: e.tensor_tensor(tmo[:], pv[:, :, 0:64], wb, ALU.mult), reads=[po, wgt], writes=[tmo])
            P.op("dve", lambda e: e.tensor_tensor(ov, ov, tmo[:], ALU.add), reads=[onsa, tmo], writes=[onsa])

    for g in range(2):
        for qb in range((NQT + 3) // 4):
            qs = slice(qb * 512, (qb + 1) * 512)
            for hh in range(4):
                h = 4 * g + hh
                pb = psA[hh % 2]

                def ms(e, pb=pb, h=h, qs=qs, g=g):
                    e.matmul(pb[0:127, :], ident_bf[0:127, 0:127], negcmp[:, qs], start=True, stop=False)
                    return e.matmul(pb[0:127, :], kcT[g][:, 0:127], qT[:, h, qs], start=False, stop=True)
                P.op("pe", ms, reads=[ident_bf, negcmp, kcT[g], qT], writes=[pb])
                P.op("act", lambda e, pb=pb, hh=hh: e.activation(Ecmp[hh][:], pb[0:127, :], AF.Exp, scale=0.125), reads=[pb], writes=[Ecmp[hh]])
            for q4 in range(4):
                qt = qb * 4 + q4
                if qt >= NQT:
                    break
                po = psA[4]

                def mc(e, po=po, q4=q4, g=g):
                    for hh in range(4):
                        r = e.matmul(po[:, hh * 97:(hh + 1) * 97], Ecmp[hh][:, q4 * 128:(q4 + 1) * 128], rcmp[g][:, :], start=True, stop=True)
                    return r
                P.op("pe", mc, reads=Ecmp + [rcmp[g]], writes=[po])
                post(po, g, qt, 0, True, 97)
                if qt >= 8:
                    pv = po[:, 0:388].rearrange("p (h w) -> p h w", h=4)
                    P.op("dve", lambda e, pv=pv: e.tensor_tensor(timp[:], pv[:, :, 65:97], den[:, 0:4].unsqueeze(2).to_broadcast([128, 4, 32]), ALU.mult),
                         reads=[po, den], writes=[timp])
                    P.op("dve", lambda e: e.reduce_sum(imp[:], timp[:].rearrange("p h j -> p j h"), AX.X), reads=[timp], writes=[imp])
                    P.op("dve", lambda e, qt=qt: e.tensor_tensor(sc1[:], imp[:], selm1[:, qt, :], ALU.mult), reads=[imp, selm1], writes=[sc1])
                    P.op("dve", lambda e, qt=qt: e.tensor_tensor(sc1[:], sc1[:], selm2[:, qt, :], ALU.add), reads=[sc1, selm2], writes=[sc1])
                    P.op("dve", lambda e: e.max(m8a[:], sc1[:]), reads=[sc1], writes=[m8a])
                    P.op("dve", lambda e: e.match_replace(sc2[:], m8a[:], sc1[:], -3.0e38), reads=[sc1, m8a], writes=[sc2])
                    P.op("dve", lambda e: e.max(m8b[:], sc2[:]), reads=[sc2], writes=[m8b])
                    P.op("dve", lambda e: e.tensor_scalar(sc2[:], sc1[:], m8b[:, 7:8], None, ALU.is_ge), reads=[sc1, m8b], writes=[sc2])
                    P.op("dve", lambda e: e.tensor_scalar(selb[:], sc2[:], -NEG, NEG, ALU.mult, ALU.add), reads=[sc2], writes=[selb])
                    P.op("pe", lambda e: e.transpose(psT[0][0:32, 0, :], selb[:], ident_bf[:]), reads=[selb, ident_bf], writes=[psT[0]])
                    P.op("act", lambda e, qt=qt, g=g: e.copy(negselT[g][:, qt, :], psT[0][0:32, 0, :]), reads=[psT[0]], writes=[negselT[g]])
        items = []
        for qt in range(NQT):
            for br, kblk, vidx in ((1, 4 + g, g), (2, 6 + g, 2 + g)):
                kts = list(range(0, qt + 1)) if br == 1 else list(range(max(0, qt - 4), qt + 1))
                for ki, kt in enumerate(kts):
                    items.append((qt, br, kblk, vidx, kt, ki, len(kts)))
        sbank = [psA[0], psA[1], psA[5]]

        def emit_scores(n, g=g):
            qt, br, kblk, vidx, kt, ki, nk = items[n]
            qsl = slice(qt * 128, (qt + 1) * 128)
            ksl = slice(kt * 128, (kt + 1) * 128)
            pb = sbank[n % 3]
            ptb = PTt[n % 3]
            cmask = None
            if kt == qt:
                cmask = negdiag4
            elif br == 2 and kt == qt - 4:
                cmask = negfar4
            smask = (br == 1 and qt >= 8)
            rd = [kT, qT]
            if cmask is not None:
                rd.append(cmask)
            if smask:
                rd += [e32, negselT[g]]

            def msc(e):
                started = False
                if cmask is not None:
                    e.matmul(pb[:], ident_bf[:], cmask[:], start=True, stop=False)
                    started = True
                elif smask:
                    e.matmul(pb[:], ident_bf[:], zer[:], start=True, stop=False)
                    started = True
                for hh in range(4):
                    cs = slice(hh * 128, (hh + 1) * 128)
                    if smask:
                        e.matmul(pb[:, cs], e32[:, ksl], negselT[g][:, qt, :], start=False, stop=False)
                    r = e.matmul(pb[:, cs], kT[:, kblk, ksl], qT[:, 4 * g + hh, qsl], start=(not started),
                                 stop=((not started) or hh == 3))
                return r
            P.op("pe", msc, reads=rd + [ident_bf, zer], writes=[pb])
            P.op("act", lambda e: e.activation(ptb[:], pb[:], AF.Exp, scale=0.125), reads=[pb], writes=[ptb])

        def emit_pv(n, g=g):
            qt, br, kblk, vidx, kt, ki, nk = items[n]
            po = psA[2 + (br - 1)]
            ptb = PTt[n % 3]
            if ki == 0:
                P.op("pe", lambda e: e.matmul(po[:, 0:260], ident_bf[:], zer[:, 0:260], start=True, stop=False),
                     reads=[ident_bf, zer], writes=[po])
            last = (ki == nk - 1)

            def mpv(e):
                for hh in range(4):
                    r = e.matmul(po[:, hh * 65:(hh + 1) * 65], ptb[:, hh * 128:(hh + 1) * 128], Vaug[:, kt, vidx, :], start=False,
                                 stop=(last and hh == 3))
                return r
            P.op("pe", mpv, reads=[ptb, Vaug], writes=[po])
            if last:
                post(po, g, qt, br, False, 65)

        LOOK = 2
        for n in range(min(LOOK, len(items))):
            emit_scores(n)
        for n in range(len(items)):
            if n + LOOK < len(items):
                emit_scores(n + LOOK)
            emit_pv(n)
    for qt in range(NQT):
        P.dma("sp", lambda e, qt=qt: [e.dma_start(out=ynsa_d[qt * 128:(qt + 1) * 128, :], in_=onsa[:, qt, :])], reads=[onsa], writes=[ynsa_d])
    P.pop()


def dn_stage(P, L):
    proj_d, ydn_d, psA = L["proj_d"], L["ydn_d"], L["psA"]
    ident_f, ones_f = L["ident_f"], L["ones_f"]
    NCH = L["dn_nchunk"]
    C = 64
    P.push()
    sb = P.sb
    psX = [P.ps("psX%d" % i, [128, 512], F32) for i in range(2)]
    banks = [psA[0:3], psA[3:6], [psX[0], psX[1], psX[0]]]
    tri = sb("tri", [C, C], F32)
    maskpos = sb("maskpos", [C, C], F32)
    strict8 = sb("strict8", [C, 512], F32)
    eye8 = sb("eye8", [C, 512], F32)
    alog = sb("alog", [C, 8], F32)
    dtb = sb("dtb", [C, 8], F32)
    grow = sb("grow", [C, 64], F32)
    P.dma("sp", lambda e: [e.dma_start(out=tri[:], in_=L["tri64_d"][:, :]),
                           e.dma_start(out=maskpos[:], in_=L["maskpos_d"][:, :]),
                           e.dma_start(out=strict8[:], in_=L["strict8_d"][:, :]),
                           e.dma_start(out=eye8[:], in_=L["eye8_d"][:, :]),
                           e.dma_start(out=alog[:], in_=L["alog_d"].partition_broadcast(C)),
                           e.dma_start(out=dtb[:], in_=L["dtb_d"].partition_broadcast(C)),
                           e.dma_start(out=grow[:], in_=L["dng_d"].partition_broadcast(C))],
          writes=[tri, maskpos, strict8, eye8, alog, dtb, grow], n=7)
    nea = sb("nea", [C, 8], F32)
    P.op("act", lambda e: e.activation(nea[:], alog[:], AF.Exp), reads=[alog], writes=[nea])
    P.op("dve", lambda e: e.tensor_scalar(nea[:], nea[:], -1.0, None, ALU.mult), reads=[nea], writes=[nea])
    Sst = sb("Sst", [C, 512], F32)
    P.op("dve", lambda e: e.memset(Sst[:], 0.0), writes=[Sst])
    tmpc = sb("tmpc", [C, 1024], F32)

    names512 = ["Gbc", "Dm", "Ds", "kT", "qT", "Nm0", "Nm1", "Mm0", "Mm1", "Rm", "attn", "attnT", "Xv", "Xk", "qd", "kd",
                "qdT", "uu", "wT", "vn", "gz", "osq", "oo"]
    names8 = ["beta", "nbeta", "ggd", "egc", "edec", "egl", "begc", "oss", "ors"]
    sets = []
    NSET = 3
    for si in range(NSET):
        T = {}
        T["yc"] = sb("yc_%d" % si, [C, 1536], F32)
        T["sm"] = sb("sm_%d" % si, [C, 16 + 512], F32)
        T["ssq"] = sb("ssq_%d" % si, [C, 16], F32)
        T["rq"] = sb("rq_%d" % si, [C, 16], F32)
        T["gcl"] = sb("gcl_%d" % si, [C, 16], F32)
        for nm in names8:
            T[nm] = sb("%s_%d" % (nm, si), [C, 8], F32)
        bf_names = ("kT", "qT", "Nm0", "Nm1", "Mm0", "Mm1", "Rm", "Xv", "Xk")
        for nm in names512:
            T[nm] = sb("%s_%d" % (nm, si), [C, 512], BF16 if nm in bf_names else F32)
        T["Nf"] = sb("Nf_%d" % si, [C, 512], F32)
        T["ps"] = banks[si]
        sets.append(T)

    def v3(buf, lo=0):
        return buf[:, lo:lo + 512].rearrange("p (h d) -> p h d", h=8)

    def bc(buf, lo=0):
        return buf[:, lo:lo + 8].unsqueeze(2).to_broadcast([C, 8, 64])

    def mm8(pb, lhs_fn, rhs_fn, reads, start=True, stop=True, n=64):
        def f(e):
            for h in range(8):
                r = e.matmul(pb[0:C, h * n:(h + 1) * n], lhs_fn(h), rhs_fn(h), start=start, stop=stop)
            return r
        P.op("pe", f, reads=reads, writes=[pb])

    def tr8(pb, src, reads):
        def f(e):
            for h in range(8):
                r = e.transpose(pb[0:C, h * 64:(h + 1) * 64], src[:, h * 64:(h + 1) * 64], ident_f[0:C, 0:C])
            return r
        P.op("pe", f, reads=reads + [ident_f], writes=[pb])

    def hs(buf):
        return lambda h: buf[:, h * 64:(h + 1) * 64]

    def chunk(c, T):
        t0 = c * C
        yc, sm, ssq, rq, gcl = T["yc"], T["sm"], T["ssq"], T["rq"], T["gcl"]
        beta, nbeta, gg, egc, edec, egl, begc, oss, ors = [T[n] for n in names8]
        (Gbc, Dm, Ds, kT, qT, Nm0, Nm1, Mm0, Mm1, Rm, attn, attnT, Xv, Xk, qd, kd, qdT, uu, wT, vn, gz, osq, oo) = [T[n] for n in names512]
        Nm, Mm = [Nm0, Nm1], [Mm0, Mm1]
        Nf = T["Nf"]
        ps = lambda i: T["ps"][i % 3]

        P.dma("sp", lambda e: [e.dma_start(out=yc[:], in_=proj_d[t0:t0 + C, O_DNQKV:O_DNQKV + 1536]),
                               e.dma_start(out=sm[:], in_=proj_d[t0:t0 + C, O_BETA:O_BETA + 528])],
              reads=[proj_d], writes=[yc, sm], n=2)
        yield
        P.op("act", lambda e: e.activation(yc[:], yc[:], AF.Silu), reads=[yc], writes=[yc])
        P.op("act", lambda e: e.activation(gz[:], sm[:, 16:528], AF.Silu), reads=[sm], writes=[gz])
        P.op("act", lambda e: e.activation(beta[:], sm[:, 0:8], AF.Sigmoid), reads=[sm], writes=[beta])
        yield
        P.op("dve", lambda e: e.tensor_tensor(tmpc[:, 0:1024], yc[:, 0:1024], yc[:, 0:1024], ALU.mult), reads=[yc], writes=[tmpc])
        P.op("dve", lambda e: e.reduce_sum(ssq[:], tmpc[:, 0:1024].rearrange("p (h d) -> p h d", h=16), AX.X), reads=[tmpc], writes=[ssq])
        yield
        P.op("dve", lambda e: e.tensor_scalar(ssq[:], ssq[:], EPS, None, ALU.add), reads=[ssq], writes=[ssq])
        P.op("act", lambda e: e.activation(rq[:], ssq[:], AF.Ln), reads=[ssq], writes=[rq])
        P.op("act", lambda e: e.activation(rq[:], rq[:], AF.Exp, scale=-0.5), reads=[rq], writes=[rq])
        yield
        P.op("dve", lambda e: e.tensor_scalar(rq[:, 0:8], rq[:, 0:8], 0.125, None, ALU.mult), reads=[rq], writes=[rq])
        P.op("dve", lambda e: e.tensor_tensor(yc[:, 0:1024].rearrange("p (h d) -> p h d", h=16), yc[:, 0:1024].rearrange("p (h d) -> p h d", h=16),
                                              rq[:, 0:16].unsqueeze(2).to_broadcast([C, 16, 64]), ALU.mult), reads=[yc, rq], writes=[yc])
        yield
        P.op("dve", lambda e: e.tensor_scalar(nbeta[:], beta[:], -1.0, None, ALU.mult), reads=[beta], writes=[nbeta])
        P.op("dve", lambda e: e.tensor_tensor(gg[:], sm[:, 8:16], dtb[:], ALU.add), reads=[sm, dtb], writes=[gg])
        P.op("act", lambda e: e.activation(gg[:], gg[:], AF.Exp), reads=[gg], writes=[gg])
        P.op("act", lambda e: e.activation(gg[:], gg[:], AF.Ln, bias=ones_f[0:C, 0:1]), reads=[gg, ones_f], writes=[gg])
        P.op("dve", lambda e: e.tensor_tensor(gg[:], gg[:], nea[:], ALU.mult), reads=[gg, nea], writes=[gg])
        yield
        pb = ps(0)

        def mg(e):
            e.matmul(pb[0:C, 0:8], tri[:], gg[:], start=True, stop=True)
            return e.matmul(pb[0:C, 8:16], ones_f[0:C, 0:C], gg[:], start=True, stop=True)
        P.op("pe", mg, reads=[tri, gg, ones_f], writes=[pb])
        P.op("dve", lambda e: e.tensor_copy(gcl[:], pb[0:C, 0:16]), reads=[pb], writes=[gcl])
        yield
        P.op("act", lambda e: e.activation(egc[:], gcl[:, 0:8], AF.Exp), reads=[gcl], writes=[egc])
        P.op("act", lambda e: e.activation(egl[:], gcl[:, 8:16], AF.Exp), reads=[gcl], writes=[egl])
        P.op("dve", lambda e: e.tensor_tensor(edec[:], gcl[:, 8:16], gcl[:, 0:8], ALU.subtract), reads=[gcl], writes=[edec])
        P.op("act", lambda e: e.activation(edec[:], edec[:], AF.Exp), reads=[edec], writes=[edec])
        P.op("dve", lambda e: e.tensor_tensor(begc[:], beta[:], egc[:], ALU.mult), reads=[beta, egc], writes=[begc])
        yield
        P.op("dve", lambda e: e.tensor_copy(v3(Gbc), bc(gg)), reads=[gg], writes=[Gbc])
        pd = ps(1)

        def md(e):
            for h in range(8):
                e.matmul(pd[0:C, h * 64:(h + 1) * 64], Gbc[:, h * 64:(h + 1) * 64], tri[:], start=True, stop=False)
                r = e.matmul(pd[0:C, h * 64:(h + 1) * 64], ident_f[0:C, 0:C], maskpos[:], start=False, stop=True)
            return r
        P.op("pe", md, reads=[Gbc, tri, ident_f, maskpos], writes=[pd])
        yield
        P.op("dve", lambda e: e.tensor_tensor(v3(Dm), pd[0:C, :].rearrange("p (h d) -> p h d", h=8), bc(gcl), ALU.subtract),
             reads=[pd, gcl], writes=[Dm])
        P.op("act", lambda e: e.activation(Dm[:], Dm[:], AF.Exp, scale=-1.0), reads=[Dm], writes=[Dm])
        P.op("dve", lambda e: e.tensor_tensor(Ds[:], Dm[:], strict8[:], ALU.mult), reads=[Dm, strict8], writes=[Ds])
        yield
        tr8(ps(2), yc[:, 512:1024], [yc])
        P.op("act", lambda e: e.copy(kT[:], ps(2)[0:C, :]), reads=[ps(2)], writes=[kT])
        yield
        tr8(ps(3), yc[:, 0:512], [yc])
        P.op("dve", lambda e: e.tensor_copy(qT[:], ps(3)[0:C, :]), reads=[ps(3)], writes=[qT])
        yield
        mm8(ps(4), hs(kT), hs(kT), [kT])
        P.op("dve", lambda e: e.tensor_tensor(v3(Nf), ps(4)[0:C, :].rearrange("p (h d) -> p h d", h=8), bc(nbeta), ALU.mult),
             reads=[ps(4), nbeta], writes=[Nf])
        P.op("dve", lambda e: e.tensor_tensor(Nf[:], Nf[:], Ds[:], ALU.mult), reads=[Nf, Ds], writes=[Nf])
        P.op("act", lambda e: e.copy(Nm[0][:], Nf[:]), reads=[Nf], writes=[Nm[0]])
        yield
        mm8(ps(5), hs(qT), hs(kT), [qT, kT])
        P.op("dve", lambda e: e.tensor_tensor(attn[:], ps(5)[0:C, :], Dm[:], ALU.mult), reads=[ps(5), Dm], writes=[attn])
        yield
        P.op("dve", lambda e: e.tensor_tensor(v3(Xv), v3(yc, 1024), bc(beta), ALU.mult), reads=[yc, beta], writes=[Xv])
        P.op("dve", lambda e: e.tensor_tensor(v3(Xk), v3(yc, 512), bc(begc), ALU.mult), reads=[yc, begc], writes=[Xk])
        yield
        P.op("dve", lambda e: e.tensor_tensor(v3(qd), v3(yc, 0), bc(egc), ALU.mult), reads=[yc, egc], writes=[qd])
        P.op("dve", lambda e: e.tensor_tensor(v3(kd), v3(yc, 512), bc(edec), ALU.mult), reads=[yc, edec], writes=[kd])
        yield
        tr8(ps(0), Nf, [Nf])
        P.op("act", lambda e: e.copy(Mm[0][:], ps(0)[0:C, :]), reads=[ps(0)], writes=[Mm[0]])
        P.op("dve", lambda e: e.tensor_tensor(Rm[:], ps(0)[0:C, :], eye8[:], ALU.add), reads=[ps(0), eye8], writes=[Rm])
        yield
        for k in range(1, 6):
            a, b_ = (k - 1) % 2, k % 2
            if k < 5:
                mm8(ps(1), hs(Nm[a]), hs(Mm[a]), [Nm[a], Mm[a]])
            mm8(ps(2), hs(Mm[a]), hs(Nm[a]), [Nm[a], Mm[a]])
            yield
            if k < 5:
                P.op("act", lambda e, b_=b_: e.copy(Mm[b_][:], ps(1)[0:C, :]), reads=[ps(1)], writes=[Mm[b_]])
            P.op("dve", lambda e, b_=b_: e.tensor_copy(Nm[b_][:], ps(2)[0:C, :]), reads=[ps(2)], writes=[Nm[b_]])
            yield
            mm8(ps(3), hs(Nm[b_]), hs(Rm), [Nm[b_], Rm])
            P.op("dve", lambda e: e.tensor_tensor(Rm[:], Rm[:], ps(3)[0:C, :], ALU.add), reads=[Rm, ps(3)], writes=[Rm])
            yield
        mm8(ps(4), hs(Rm), hs(Xv), [Rm, Xv])
        P.op("act", lambda e: e.copy(uu[:], ps(4)[0:C, :]), reads=[ps(4)], writes=[uu])
        yield
        mm8(ps(5), hs(Xk), hs(Rm), [Rm, Xk])
        P.op("dve", lambda e: e.tensor_copy(wT[:], ps(5)[0:C, :]), reads=[ps(5)], writes=[wT])
        yield
        tr8(ps(0), attn, [attn])
        P.op("act", lambda e: e.copy(attnT[:], ps(0)[0:C, :]), reads=[ps(0)], writes=[attnT])
        yield
        tr8(ps(1), qd, [qd])
        P.op("dve", lambda e: e.tensor_copy(qdT[:], ps(1)[0:C, :]), reads=[ps(1)], writes=[qdT])
        P.op("dve", lambda e: e.tensor_tensor(v3(gz), v3(gz), grow[:, 0:64].unsqueeze(1).to_broadcast([C, 8, 64]), ALU.mult), reads=[gz, grow], writes=[gz])
        yield
        mm8(ps(2), hs(wT), hs(Sst), [wT, Sst])
        P.op("dve", lambda e: e.tensor_tensor(vn[:], uu[:], ps(2)[0:C, :], ALU.subtract), reads=[uu, ps(2)], writes=[vn])
        yield
        po = ps(3)

        def mo(e):
            for h in range(8):
                e.matmul(po[0:C, h * 64:(h + 1) * 64], qdT[:, h * 64:(h + 1) * 64], Sst[:, h * 64:(h + 1) * 64], start=True, stop=False)
                r = e.matmul(po[0:C, h * 64:(h + 1) * 64], attnT[:, h * 64:(h + 1) * 64], vn[:, h * 64:(h + 1) * 64], start=False, stop=True)
            return r
        P.op("pe", mo, reads=[qdT, Sst, attnT, vn], writes=[po])
        mm8(ps(4), hs(kd), hs(vn), [kd, vn])
        yield
        P.op("dve", lambda e: e.tensor_tensor(v3(Sst), v3(Sst), bc(egl), ALU.mult), reads=[Sst, egl], writes=[Sst])
        P.op("dve", lambda e: e.tensor_tensor(Sst[:], Sst[:], ps(4)[0:C, :], ALU.add), reads=[Sst, ps(4)], writes=[Sst])
        yield
        P.op("dve", lambda e: e.tensor_copy(oo[:], po[0:C, :]), reads=[po], writes=[oo])
        P.op("dve", lambda e: e.tensor_tensor(osq[:], oo[:], oo[:], ALU.mult), reads=[oo], writes=[osq])
        P.op("dve", lambda e: e.reduce_sum(oss[:], v3(osq), AX.X), reads=[osq], writes=[oss])
        yield
        P.op("dve", lambda e: e.tensor_scalar(oss[:], oss[:], 1.0 / 64, EPS, ALU.mult, ALU.add), reads=[oss], writes=[oss])
        P.op("act", lambda e: e.activation(ors[:], oss[:], AF.Ln), reads=[oss], writes=[ors])
        P.op("act", lambda e: e.activation(ors[:], ors[:], AF.Exp, scale=-0.5), reads=[ors], writes=[ors])
        yield
        P.op("dve", lambda e: e.tensor_tensor(v3(oo), v3(oo), bc(ors), ALU.mult), reads=[oo, ors], writes=[oo])
        P.op("dve", lambda e: e.tensor_tensor(oo[:], oo[:], gz[:], ALU.mult), reads=[oo, gz], writes=[oo])
        P.dma("sp", lambda e: [e.dma_start(out=ydn_d[t0:t0 + C, :], in_=oo[:])], reads=[oo], writes=[ydn_d])
        yield

    gens = {}
    nxt = 0
    STAG = 10
    for si in range(NSET):
        if nxt < NCH:
            gens[si] = chunk(nxt, sets[si])
            nxt += 1
            if si < NSET - 1:
                for _ in range(STAG):
                    for sj in list(gens.keys()):
                        try:
                            next(gens[sj])
                        except StopIteration:
                            del gens[sj]
    while gens:
        for si in list(gens.keys()):
            try:
                next(gens[si])
            except StopIteration:
                del gens[si]
                if nxt < NCH:
                    gens[si] = chunk(nxt, sets[si])
                    nxt += 1
    P.pop()


def core_inputs(inputs, core, nseq=BPC):
    m = {}
    b0 = core * nseq
    g = lambda k: np.ascontiguousarray(np.asarray(inputs[k], dtype=np.float32))
    m["x"] = np.ascontiguousarray(inputs["x"][b0:b0 + nseq])
    m["c"] = np.ascontiguousarray(inputs["c"][b0:b0 + nseq])
    m["w_ada"] = g("w_ada")[0]
    m["b_ada"] = g("b_ada")[0].reshape(1, -1)
    m["g_norm_mix"] = g("g_norm_mix")[0].reshape(1, -1)
    m["g_norm_ffn"] = g("g_norm_ffn")[0].reshape(1, -1)
    m["final_norm_g"] = g("final_norm_g").reshape(1, -1)
    m["w_in"] = g("w_in")[0]
    m["w_branch"] = g("w_branch")[0]
    m["w_out"] = g("w_out")[0]
    m["router_w"] = g("router_w")[0]
    m["router_b"] = g("router_b")[0].reshape(1, -1)
    m["exp_w1"] = g("exp_w1")[0]
    m["exp_b1"] = g("exp_b1")[0]
    m["exp_w2"] = g("exp_w2")[0]
    m["exp_b2"] = g("exp_b2")[0]
    m["cmp_pe_k"] = g("cmp_pe_k")[0]
    m["cmp_pe_v"] = g("cmp_pe_v")[0]
    m["cmp_w1"] = g("cmp_w1")[0]
    m["cmp_b1"] = g("cmp_b1")[0]
    m["cmp_w2"] = g("cmp_w2")[0]
    m["dn_conv_w"] = g("dn_conv_w")[0].reshape(1, -1)
    m["dn_a_log"] = g("dn_a_log")[0].reshape(1, -1)
    m["dn_dt_bias"] = g("dn_dt_bias")[0].reshape(1, -1)
    m["dn_norm_g"] = g("dn_norm_g")[0].reshape(1, -1)
    m.update(host_consts())
    return m


def kernel(**inputs):
    nc = build()
    in_maps = [core_inputs(inputs, c) for c in range(NCORES)]
    res = run_bass_kernel_spmd(nc, in_maps, core_ids=list(range(NCORES)))
    return np.concatenate([r["out"] for r in res.results], axis=0).astype(np.float32)
```
